# Optimizing a Trainium2 kernel written in Bass

```python
import math
import jax, jax.numpy as jnp
from jax import lax
import numpy as np

D_MODEL = 1024
BATCH = 1
SEQ = 16384
DEPTH = 4

GRID_W = 64
CTX_LEN = 256
N_MIXERS = 2
N_GDN_LAYERS = (DEPTH + N_MIXERS - 1) // N_MIXERS
N_DIFF_LAYERS = DEPTH // N_MIXERS
GDN_HEAD_DIM = 128
GDN_QK_HEADS = D_MODEL // GDN_HEAD_DIM
GDN_V_HEADS = 2 * GDN_QK_HEADS
GDN_QK_DIM = GDN_QK_HEADS * GDN_HEAD_DIM
GDN_V_DIM = GDN_V_HEADS * GDN_HEAD_DIM
GDN_CONV_DIM = 2 * GDN_QK_DIM + GDN_V_DIM
GDN_IN_DIM = GDN_CONV_DIM + GDN_V_DIM + 4 * GDN_V_HEADS
GDN_CONV_W = 5
GDN_CHUNK = 64
DIFF_HEADS = D_MODEL // 128
DIFF_HEAD_DIM = 64
DIFF_V_DIM = 2 * DIFF_HEAD_DIM
ROPE_THETA = 10000.0
Q_BLOCK = 128
N_EXPERTS = 32
TOP_K = 4
D_FF = D_MODEL
SWIGLU_LIMIT = 7.0
SWIGLU_ALPHA = 1.702
MOE_BLOCK = 128
NORM_EPS = 1e-5
DEEPNORM_ALPHA = (2 * DEPTH) ** 0.25
DEEPNORM_BETA = (8 * DEPTH) ** -0.25

kernel_name = 'hybrid_gdn_diffattn_moe_dit'


def _layer_norm(x, w, b):
    xf = x.astype(jnp.float32)
    mu = jnp.mean(xf, -1, keepdims=True)
    var = jnp.mean(jnp.square(xf - mu), -1, keepdims=True)
    return ((xf - mu) * lax.rsqrt(var + NORM_EPS)).astype(x.dtype) * w + b


def _rms_norm(x, w):
    xf = x.astype(jnp.float32)
    return xf * lax.rsqrt(jnp.mean(jnp.square(xf), -1, keepdims=True) + NORM_EPS) * w


def _l2_normalize(x):
    xf = x.astype(jnp.float32)
    return xf * lax.rsqrt(jnp.sum(jnp.square(xf), -1, keepdims=True) + 1e-6)


def _centred_depthwise_conv(u, w):
    pad = GDN_CONV_W // 2
    return lax.conv_general_dilated(u, w[:, None, :].astype(u.dtype), window_strides=(1,),
                                    padding=[(pad, pad)], dimension_numbers=('NWC', 'WIO', 'NWC'),
                                    feature_group_count=u.shape[-1])


def _gdn_features(u, w_in, conv_w, a_log, dt_bias):
    B, L, _ = u.shape
    p = u @ w_in
    qkv = jax.nn.silu(_centred_depthwise_conv(p[..., :GDN_CONV_DIM], conv_w))
    z = p[..., GDN_CONV_DIM:GDN_CONV_DIM + GDN_V_DIM].reshape(B, L, GDN_V_HEADS, GDN_HEAD_DIM)
    ab = p[..., GDN_CONV_DIM + GDN_V_DIM:].astype(jnp.float32).reshape(B, L, 2, 2, GDN_V_HEADS)
    rep = GDN_V_HEADS // GDN_QK_HEADS
    q = jnp.repeat(_l2_normalize(qkv[..., :GDN_QK_DIM].reshape(B, L, GDN_QK_HEADS, GDN_HEAD_DIM)), rep, axis=2)
    k = jnp.repeat(_l2_normalize(qkv[..., GDN_QK_DIM:2 * GDN_QK_DIM].reshape(B, L, GDN_QK_HEADS, GDN_HEAD_DIM)), rep, axis=2)
    v = qkv[..., 2 * GDN_QK_DIM:].reshape(B, L, GDN_V_HEADS, GDN_HEAD_DIM)
    g = -jnp.exp(a_log.astype(jnp.float32)) * jax.nn.softplus(ab[:, :, 0] + dt_bias.astype(jnp.float32))
    beta = jax.nn.sigmoid(ab[:, :, 1])
    return q, k, v, z, g, beta


def _chunked_gated_delta(q, k, v, g, beta, s0):
    B, L, H, DK = k.shape
    DV = v.shape[-1]
    C = GDN_CHUNK
    n = L // C

    def blocks(t):
        t = t.astype(jnp.float32).reshape(B, n, C, H, *t.shape[3:])
        return jnp.moveaxis(t, (1, 3), (0, 2))

    q = blocks(q) * DK ** -0.5
    k, v = blocks(k), blocks(v)
    g = jnp.cumsum(blocks(g), axis=-1)
    beta = blocks(beta)
    incl = jnp.tril(jnp.ones((C, C), bool))
    strict = jnp.tril(jnp.ones((C, C), bool), -1)
    decay = jnp.exp(jnp.where(incl, g[..., :, None] - g[..., None, :], -jnp.inf))
    kk = jnp.einsum('nbhid,nbhjd->nbhij', k, k)
    a_mat = jnp.where(strict, beta[..., :, None] * kk * decay, 0.0) + jnp.eye(C, dtype=jnp.float32)
    rhs = jnp.concatenate([v * beta[..., None], k * (beta * jnp.exp(g))[..., None]], -1)
    sol = lax.linalg.triangular_solve(a_mat, rhs, left_side=True, lower=True, unit_diagonal=True)
    u_new, w_dec = sol[..., :DV], sol[..., DV:]
    qk = jnp.einsum('nbhid,nbhjd->nbhij', q, k) * decay
    q_dec = q * jnp.exp(g)[..., None]
    g_last = g[..., -1]
    k_dec = k * jnp.exp(g_last[..., None] - g)[..., None]

    def step(s, blk):
        qk_i, q_i, u_i, w_i, k_i, gl_i = blk
        v_new = u_i - jnp.einsum('bhck,bhkv->bhcv', w_i, s)
        o = jnp.einsum('bhck,bhkv->bhcv', q_i, s) + jnp.einsum('bhij,bhjv->bhiv', qk_i, v_new)
        s = s * jnp.exp(gl_i)[..., None, None] + jnp.einsum('bhck,bhcv->bhkv', k_i, v_new)
        return s, o

    s_fin, o = lax.scan(step, s0, (qk, q_dec, u_new, w_dec, k_dec, g_last))
    o = jnp.moveaxis(o, (0, 2), (1, 3)).reshape(B, L, H, DV)
    return s_fin, o


def _gdn_direction(fc, fl, d):
    rev = (lambda t: jnp.flip(t, axis=1)) if d == 1 else (lambda t: t)

    def run(f, s0):
        q, k, v, _, g, beta = f
        s, o = _chunked_gated_delta(rev(q), rev(k), rev(v), rev(g[:, :, d]), rev(beta[:, :, d]), s0)
        return s, rev(o)

    B = fl[0].shape[0]
    s0 = jnp.zeros((B, GDN_V_HEADS, GDN_HEAD_DIM, GDN_HEAD_DIM), jnp.float32)
    s_ctx, o_ctx = run(fc, s0)
    _, o_lat = run(fl, s_ctx)
    return o_ctx, o_lat


def _gated_deltanet(u_ctx, u_lat, w_in, conv_w, a_log, dt_bias, norm_w, w_out):
    fc = _gdn_features(u_ctx, w_in, conv_w, a_log, dt_bias)
    fl = _gdn_features(u_lat, w_in, conv_w, a_log, dt_bias)
    oc_f, ol_f = _gdn_direction(fc, fl, 0)
    oc_b, ol_b = _gdn_direction(fc, fl, 1)

    def out(o, z, u):
        B, L, _ = u.shape
        y = _rms_norm(o, norm_w) * jax.nn.silu(z.astype(jnp.float32))
        return y.astype(u.dtype).reshape(B, L, GDN_V_DIM) @ w_out

    return out(oc_f + oc_b, fc[3], u_ctx), out(ol_f + ol_b, fl[3], u_lat)


def _axial_rope(n_lat):
    rows = n_lat // GRID_W
    row = jnp.repeat(jnp.arange(rows, dtype=jnp.float32), GRID_W)
    col = jnp.tile(jnp.arange(GRID_W, dtype=jnp.float32), rows)
    axis_dim = DIFF_HEAD_DIM // 2
    inv_freq = ROPE_THETA ** (-jnp.arange(0, axis_dim, 2, dtype=jnp.float32) / axis_dim)
    ang_r = row[:, None] * inv_freq
    ang_c = col[:, None] * inv_freq
    ang = jnp.concatenate([ang_r, ang_r, ang_c, ang_c], -1)
    return jnp.cos(ang), jnp.sin(ang)


def _apply_axial_rope(x, cos, sin):
    x1, x2, x3, x4 = jnp.split(x, 4, axis=-1)
    rot = jnp.concatenate([-x2, x1, -x4, x3], -1)
    return (x * cos[None, :, None, :] + rot * sin[None, :, None, :]).astype(x.dtype)


def _diff_attention(u_ctx, u_lat, w_in, lam, subln_w, w_out, lambda_init, need_ctx_out):
    B, L_lat, D = u_lat.shape

    def qkv(u):
        L = u.shape[1]
        q, k, v = jnp.split(u @ w_in, 3, axis=-1)
        return (q.reshape(B, L, 2 * DIFF_HEADS, DIFF_HEAD_DIM), k.reshape(B, L, 2 * DIFF_HEADS, DIFF_HEAD_DIM),
                v.reshape(B, L, DIFF_HEADS, DIFF_V_DIM))

    qc, kc, vc = qkv(u_ctx)
    ql, kl, vl = qkv(u_lat)
    cos, sin = _axial_rope(L_lat)
    ql, kl = _apply_axial_rope(ql, cos, sin), _apply_axial_rope(kl, cos, sin)
    lf = lam.astype(jnp.float32)
    lam_full = jnp.exp(jnp.sum(lf[0] * lf[1])) - jnp.exp(jnp.sum(lf[2] * lf[3])) + lambda_init

    def attend(q, k, v):
        Lq, Lk = q.shape[1], k.shape[1]
        s = jnp.einsum('bqhd,bkhd->bhqk', q, k).astype(jnp.float32) * DIFF_HEAD_DIM ** -0.5
        p = jax.nn.softmax(s, axis=-1).reshape(B, DIFF_HEADS, 2, Lq, Lk)
        a = (p[:, :, 0] - lam_full * p[:, :, 1]).astype(v.dtype)
        o = jnp.einsum('bhqk,bkhe->bqhe', a, v)
        o = _rms_norm(o, subln_w) * (1.0 - lambda_init)
        return o.astype(v.dtype).reshape(B, Lq, DIFF_HEADS * DIFF_V_DIM)

    keys = jnp.concatenate([kc, kl], axis=1)
    vals = jnp.concatenate([vc, vl], axis=1)
    qb = jnp.moveaxis(ql.reshape(B, L_lat // Q_BLOCK, Q_BLOCK, 2 * DIFF_HEADS, DIFF_HEAD_DIM), 1, 0)
    ob = lax.map(lambda q_blk: attend(q_blk, keys, vals), qb)
    y_lat = jnp.moveaxis(ob, 0, 1).reshape(B, L_lat, D) @ w_out
    y_ctx = attend(qc, kc, vc) @ w_out if need_ctx_out else None
    return y_ctx, y_lat


def _moe(x2d, router_w, router_b, w_gu, b_gu, w_dn, b_dn):
    N, D = x2d.shape
    logits = (x2d @ router_w + router_b).astype(jnp.float32)
    top_val, top_idx = lax.top_k(logits, TOP_K)
    gates = jax.nn.softmax(top_val, axis=-1).astype(x2d.dtype)
    NK = N * TOP_K
    flat_e = top_idx.reshape(NK)
    order = jnp.argsort(flat_e)
    sorted_e = flat_e[order]
    counts = jnp.zeros((N_EXPERTS,), jnp.int32).at[flat_e].add(1)
    padded = (counts + MOE_BLOCK - 1) // MOE_BLOCK * MOE_BLOCK
    pad_end = jnp.cumsum(padded)
    pad_start = pad_end - padded
    start = jnp.cumsum(counts) - counts
    dest_sorted = pad_start[sorted_e] + (jnp.arange(NK, dtype=jnp.int32) - start[sorted_e])
    dest = jnp.zeros((NK,), jnp.int32).at[order].set(dest_sorted)
    n_blocks = -(-NK // MOE_BLOCK) + N_EXPERTS
    n_rows = n_blocks * MOE_BLOCK
    row_tok = jnp.full((n_rows,), N, jnp.int32).at[dest].set(jnp.arange(NK, dtype=jnp.int32) // TOP_K)
    block_e = jnp.minimum(jnp.searchsorted(pad_end, jnp.arange(n_blocks, dtype=jnp.int32) * MOE_BLOCK, side='right'),
                          N_EXPERTS - 1)
    x_pad = jnp.concatenate([x2d, jnp.zeros((1, D), x2d.dtype)], axis=0)
    xb = x_pad[row_tok].reshape(n_blocks, MOE_BLOCK, D)

    def expert_block(args):
        xe, e = args
        h = xe @ w_gu[e] + b_gu[e]
        gate = jnp.minimum(h[:, :D_FF], SWIGLU_LIMIT)
        up = jnp.clip(h[:, D_FF:], -SWIGLU_LIMIT, SWIGLU_LIMIT)
        glu = gate * jax.nn.sigmoid(gate * SWIGLU_ALPHA)
        return ((up + 1.0) * glu) @ w_dn[e] + b_dn[e]

    yb = lax.map(expert_block, (xb, block_e)).reshape(n_rows, D)
    y = yb[dest].reshape(N, TOP_K, D)
    return jnp.einsum('nk,nkd->nd', gates, y)


def setup_inputs(seed: int = 0) -> dict:
    key = jax.random.key(seed)
    ks = jax.random.split(key, 24)
    f32 = jnp.float32

    def nrm(k, shape, scale):
        return jax.random.normal(k, shape, f32) * scale

    G, K = N_GDN_LAYERS, N_DIFF_LAYERS
    dt = jnp.exp(jax.random.uniform(ks[11], (G, 2, GDN_V_HEADS), f32, math.log(1e-3), math.log(1e-1)))
    return {
        'x': nrm(ks[0], (BATCH, SEQ, D_MODEL), 1.0),
        'c': nrm(ks[1], (BATCH, D_MODEL), 1.0),
        'ctx': nrm(ks[2], (BATCH, CTX_LEN, D_MODEL), 1.0),
        'c_ctx': nrm(ks[3], (D_MODEL,), 1.0),
        'ada_w': nrm(ks[4], (DEPTH, D_MODEL, 6 * D_MODEL), 0.5 * D_MODEL ** -0.5),
        'ada_b': nrm(ks[5], (DEPTH, 6 * D_MODEL), 0.02),
        'ln_w': 1.0 + nrm(ks[6], (DEPTH, 2, D_MODEL), 0.02),
        'ln_b': nrm(ks[7], (DEPTH, 2, D_MODEL), 0.02),
        'gdn_w_in': nrm(ks[8], (G, D_MODEL, GDN_IN_DIM), D_MODEL ** -0.5),
        'gdn_conv_w': nrm(ks[9], (G, GDN_CONV_W, GDN_CONV_DIM), GDN_CONV_W ** -0.5),
        'gdn_a_log': jnp.log(jax.random.uniform(ks[10], (G, 2, GDN_V_HEADS), f32, 1.0, 16.0)),
        'gdn_dt_bias': dt + jnp.log(-jnp.expm1(-dt)),
        'gdn_norm_w': 1.0 + nrm(ks[12], (G, GDN_HEAD_DIM), 0.02),
        'gdn_w_out': nrm(ks[13], (G, GDN_V_DIM, D_MODEL), DEEPNORM_BETA * GDN_V_DIM ** -0.5),
        'diff_w_in': nrm(ks[14], (K, D_MODEL, 3 * D_MODEL), D_MODEL ** -0.5),
        'diff_lambda': nrm(ks[15], (K, 4, DIFF_HEAD_DIM), 0.1),
        'diff_subln_w': 1.0 + nrm(ks[16], (K, DIFF_V_DIM), 0.02),
        'diff_w_out': nrm(ks[17], (K, DIFF_HEADS * DIFF_V_DIM, D_MODEL), DEEPNORM_BETA * D_MODEL ** -0.5),
        'router_w': nrm(ks[18], (DEPTH, D_MODEL, N_EXPERTS), D_MODEL ** -0.5),
        'router_b': nrm(ks[19], (DEPTH, N_EXPERTS), 0.01),
        'moe_w_gate_up': nrm(ks[20], (DEPTH, N_EXPERTS, D_MODEL, 2 * D_FF), D_MODEL ** -0.5),
        'moe_b_gate_up': nrm(ks[21], (DEPTH, N_EXPERTS, 2 * D_FF), 0.02),
        'moe_w_down': nrm(ks[22], (DEPTH, N_EXPERTS, D_FF, D_MODEL), DEEPNORM_BETA * D_FF ** -0.5),
        'moe_b_down': nrm(ks[23], (DEPTH, N_EXPERTS, D_MODEL), 0.02),
    }


def reference(x, c, ctx, c_ctx, ada_w, ada_b, ln_w, ln_b, gdn_w_in, gdn_conv_w, gdn_a_log, gdn_dt_bias,
              gdn_norm_w, gdn_w_out, diff_w_in, diff_lambda, diff_subln_w, diff_w_out, router_w, router_b,
              moe_w_gate_up, moe_b_gate_up, moe_w_down, moe_b_down):
    B, L_lat, D = x.shape
    L_ctx = ctx.shape[1]
    h_lat, h_ctx = x, ctx
    s_lat = jax.nn.silu(c)[:, None, :]
    s_ctx = jax.nn.silu(c_ctx)[None, None, :]
    for i in range(DEPTH):
        last = i == DEPTH - 1
        j = i // N_MIXERS
        m_lat = jnp.split(s_lat @ ada_w[i] + ada_b[i], 6, axis=-1)
        m_ctx = jnp.split(s_ctx @ ada_w[i] + ada_b[i], 6, axis=-1)
        u_lat = h_lat * (1.0 + m_lat[1]) + m_lat[0]
        u_ctx = h_ctx * (1.0 + m_ctx[1]) + m_ctx[0]
        if i % N_MIXERS == 0:
            y_ctx, y_lat = _gated_deltanet(u_ctx, u_lat, gdn_w_in[j], gdn_conv_w[j], gdn_a_log[j], gdn_dt_bias[j],
                                           gdn_norm_w[j], gdn_w_out[j])
        else:
            lambda_init = 0.8 - 0.6 * math.exp(-0.3 * i)
            y_ctx, y_lat = _diff_attention(u_ctx, u_lat, diff_w_in[j], diff_lambda[j], diff_subln_w[j], diff_w_out[j],
                                           lambda_init, not last)
        h_lat = _layer_norm(DEEPNORM_ALPHA * h_lat + m_lat[2] * y_lat, ln_w[i, 0], ln_b[i, 0])
        v_lat = h_lat * (1.0 + m_lat[4]) + m_lat[3]
        if last:
            f_lat = _moe(v_lat.reshape(-1, D), router_w[i], router_b[i], moe_w_gate_up[i], moe_b_gate_up[i],
                         moe_w_down[i], moe_b_down[i]).reshape(B, L_lat, D)
        else:
            h_ctx = _layer_norm(DEEPNORM_ALPHA * h_ctx + m_ctx[2] * y_ctx, ln_w[i, 0], ln_b[i, 0])
            v_ctx = h_ctx * (1.0 + m_ctx[4]) + m_ctx[3]
            v_all = jnp.concatenate([v_ctx, v_lat], axis=1).reshape(-1, D)
            f_all = _moe(v_all, router_w[i], router_b[i], moe_w_gate_up[i], moe_b_gate_up[i],
                         moe_w_down[i], moe_b_down[i]).reshape(B, L_ctx + L_lat, D)
            f_ctx, f_lat = f_all[:, :L_ctx], f_all[:, L_ctx:]
            h_ctx = _layer_norm(DEEPNORM_ALPHA * h_ctx + m_ctx[5] * f_ctx, ln_w[i, 1], ln_b[i, 1])
        h_lat = _layer_norm(DEEPNORM_ALPHA * h_lat + m_lat[5] * f_lat, ln_w[i, 1], ln_b[i, 1])
    return h_lat
```

```python
import math
from contextlib import ExitStack
import numpy as np
import ml_dtypes
import concourse.bass as bass
import concourse.mybir as mybir
from concourse.bass_utils import run_bass_kernel_spmd

F32 = mybir.dt.float32
BF16 = mybir.dt.bfloat16
AF = mybir.ActivationFunctionType
ALU = mybir.AluOpType
AX = mybir.AxisListType
NPBF = ml_dtypes.bfloat16

NCORES = 8
D = 1024
KC = D // 128
CTX = 256
DEPTH = 4
NEXP = 32
EPC = NEXP // NCORES
ALPHA = (2 * DEPTH) ** 0.25
EPS = 1e-5

LAUNCHES = [0]
PENG = 'pool'
DBG = {}


class Buf:
    def __init__(self, t, name, space):
        self.t = t
        self.name = name
        self.space = space
        self.lw = None
        self.rd = []
        self.dsem = None
        self.dcnt = 0

    def __getitem__(self, idx):
        return self.t[idx]


class Prog:
    ENG = ['pe', 'act', 'dve', 'pool', 'sp']

    def __init__(self):
        self.nc = bass.Bass("TRN2", target_bir_lowering=False)
        self.st = ExitStack()
        self.q = {e: [] for e in self.ENG}
        self.cnt = {e: 0 for e in self.ENG}
        self.seen = {e: {} for e in self.ENG}
        self.esem = {e: self.st.enter_context(self.nc.semaphore("es_" + e)) for e in self.ENG}
        self.outs = []
        self.nid = 0

    def sb(self, name, shape, dt=F32):
        t = self.st.enter_context(self.nc.sbuf_tensor(name, list(shape), dt))
        return Buf(t, name, 'sb')

    def ps(self, name, shape, dt=F32):
        t = self.st.enter_context(self.nc.psum_tensor(name, list(shape), dt))
        return Buf(t, name, 'ps')

    def din(self, name, shape, dt=F32):
        return Buf(self.nc.dram_tensor(name, list(shape), dt, kind="ExternalInput").ap(), name, 'dram')

    def dout(self, name, shape, dt=F32):
        b = Buf(self.nc.dram_tensor(name, list(shape), dt, kind="ExternalOutput").ap(), name, 'dram')
        self.outs.append(b)
        return b

    def dscratch(self, name, shape, dt=F32):
        return Buf(self.nc.dram_tensor(name, list(shape), dt).ap(), name, 'dram')

    def _need(self, eng, tok, waits, raw=False):
        if tok is None:
            return
        if tok[0] == 'e':
            if tok[1] == eng and (eng == 'pe' or not raw):
                return
            key = ('e', tok[1])
            sem = self.esem[tok[1]]
        else:
            key = ('d', id(tok[1]))
            sem = tok[1].dsem
        val = tok[2]
        if self.seen[eng].get(key, 0) >= val:
            return
        self.seen[eng][key] = val
        waits.append((sem, val))

    def op(self, eng, fn, reads=(), writes=()):
        waits = []
        for b in reads:
            self._need(eng, b.lw, waits, raw=True)
            if b.space == 'ps':
                for r in b.rd:
                    self._need(eng, r, waits)
        for b in writes:
            self._need(eng, b.lw, waits)
            for r in b.rd:
                self._need(eng, r, waits)
        self.cnt[eng] += 1
        tok = ('e', eng, self.cnt[eng])
        for b in reads:
            b.rd.append(tok)
            if len(b.rd) > 64:
                b.rd = b.rd[-64:] if False else b.rd
        for b in writes:
            b.lw = tok
            b.rd = []
        self.q[eng].append((waits, fn, None))

    def dma(self, qeng, dst, dst_ap, src, src_ap, disjoint=False):
        waits = []
        self._need(qeng, src.lw, waits)
        if not disjoint:
            self._need(qeng, dst.lw, waits)
            for r in dst.rd:
                self._need(qeng, r, waits)
        if dst.dsem is None:
            dst.dsem = self.st.enter_context(self.nc.semaphore("ds_%s_%d" % (dst.name, self.nid)))
            self.nid += 1
        dst.dcnt += 16
        tok = ('d', dst, dst.dcnt)
        src.rd.append(tok)
        dst.lw = tok
        dst.rd = []
        self.q[qeng].append((waits, lambda e: e.dma_start(out=dst_ap, in_=src_ap), dst))

    def build(self):
        nc = self.nc
        fin = []
        for b in self.outs:
            if b.dsem is not None:
                fin.append((b.dsem, b.dcnt))
        with nc.Block() as block:
            def mk(ename):
                def body(engine):
                    for waits, fn, dbuf in self.q[ename]:
                        for sem, val in waits:
                            engine.wait_ge(sem, val)
                        ins = fn(engine)
                        if dbuf is None:
                            ins.then_inc(self.esem[ename], 1)
                        else:
                            ins.then_inc(dbuf.dsem, 16)
                    if ename == 'sp':
                        for sem, val in fin:
                            engine.wait_ge(sem, val)
                return body
            block.tensor(mk('pe'))
            block.scalar(mk('act'))
            block.vector(mk('dve'))
            block.gpsimd(mk('pool'))
            block.sync(mk('sp'))
        self.st.close()
        return nc

    def mm(self, out, out_ap, lhsT, lhsT_ap, rhs, rhs_ap, start, stop):
        rd = [lhsT, rhs] + ([] if start else [out])
        self.op('pe', lambda e: e.matmul(out_ap, lhsT=lhsT_ap, rhs=rhs_ap, start=start, stop=stop),
                reads=rd, writes=[out])

    def tr(self, out, out_ap, in_, in_ap, ident, ident_ap):
        self.op('pe', lambda e: e.transpose(out_ap, in_ap, ident_ap), reads=[in_, ident], writes=[out])

    def run(self, in_maps):
        import time as _t
        t0 = _t.time()
        nc = self.build()
        t1 = _t.time()
        LAUNCHES[0] += 1
        res = run_bass_kernel_spmd(nc, in_maps, core_ids=list(range(NCORES)))
        t2 = _t.time()
        if DBG.get('timing', 1):
            nb = sum(v.nbytes for m_ in in_maps for v in m_.values())
            ob = sum(v.nbytes for r_ in res.results for v in r_.values())
            print("[launch %d] build %.1fs run %.1fs in %.0fMB out %.0fMB ninstr %d" % (
                LAUNCHES[0], t1 - t0, t2 - t1, nb / 1e6, ob / 1e6, sum(len(q) for q in self.q.values())), flush=True)
        return res.results


def consts_np():
    c = {}
    c['ident_f'] = np.eye(128, dtype=np.float32)
    c['ident_b'] = np.eye(128, dtype=np.float32).astype(NPBF)
    return c


def launch_mods(c, c_ctx, ada_w, ada_b):
    NW = 6 * D // NCORES
    P = Prog()
    cs_d = P.din("cs", [128, KC * 2])
    w_d = P.din("w", [DEPTH * KC, 128, NW])
    b_d = P.din("b", [1, DEPTH * NW])
    out_d = P.dout("mods", [2, DEPTH * NW])
    cs = P.sb("cs_s", [128, KC * 2])
    sg = P.sb("sg_s", [128, KC * 2])
    s = P.sb("s_s", [128, KC * 2])
    bs = P.sb("b_s", [1, DEPTH * NW])
    ones = P.sb("ones", [1, 2])
    osb = P.sb("o_s", [2, DEPTH * NW])
    wt = [P.sb("w%d" % i, [128, NW]) for i in range(4)]
    pp = [P.ps("pp%d" % i, [2, 512]) for i in range(4)]
    P.dma('sp', cs, cs[:], cs_d, cs_d[:, :])
    P.dma('sp', bs, bs[:], b_d, b_d[:, :])
    P.op('dve', lambda e: e.memset(ones[:], 1.0), writes=[ones])
    P.op('act', lambda e: e.activation(out=sg[:], in_=cs[:], func=AF.Sigmoid), reads=[cs], writes=[sg])
    P.op('dve', lambda e: e.tensor_tensor(out=s[:], in0=cs[:], in1=sg[:], op=ALU.mult), reads=[cs, sg], writes=[s])
    n = 0
    for l in range(DEPTH):
        for k in range(KC):
            w = wt[n % 4]
            P.dma('sp', w, w[:], w_d, w_d[l * KC + k, :, :])
            for h in range(2):
                pt = pp[(l % 2) * 2 + h]
                P.mm(pt, pt[:, 0:384], s, s[:, 2 * k:2 * k + 2], w, w[:, h * 384:(h + 1) * 384], k == 0, False)
            n += 1
        for h in range(2):
            pt = pp[(l % 2) * 2 + h]
            c0 = l * NW + h * 384
            P.mm(pt, pt[:, 0:384], ones, ones[:, :], bs, bs[:, c0:c0 + 384], False, True)
            P.op('dve', lambda e, pt=pt, c0=c0: e.tensor_copy(out=osb[:, c0:c0 + 384], in_=pt[:, 0:384]),
                 reads=[pt], writes=[osb])
    P.dma('sp', out_d, out_d[:, :], osb, osb[:])
    cs_np = np.ascontiguousarray(
        np.stack([c.reshape(KC, 128), c_ctx.reshape(KC, 128)], -1).transpose(1, 0, 2).reshape(128, KC * 2))
    in_maps = []
    for r in range(NCORES):
        wsh = ada_w[:, :, r * NW:(r + 1) * NW].reshape(DEPTH * KC, 128, NW)
        bsh = ada_b[:, r * NW:(r + 1) * NW].reshape(1, DEPTH * NW)
        in_maps.append({"cs": cs_np, "w": np.ascontiguousarray(wsh), "b": np.ascontiguousarray(bsh)})
    res = P.run(in_maps)
    mods = np.zeros((DEPTH, 2, 6 * D), np.float32)
    for r in range(NCORES):
        o = res[r]["mods"].reshape(2, DEPTH, NW)
        for l in range(DEPTH):
            mods[l, :, r * NW:(r + 1) * NW] = o[:, l, :]
    return mods


def _ln_mod(P, W, src, lnw, lnb, sc1, sh, out_h, out_u):
    ag, rstd, tmp, junk = W['ag'], W['rstd'], W['tmp'], W['junk']
    P.op('act', lambda e: e.activation(out=junk[:], in_=src[:], func=AF.Copy, accum_out=ag[:, 0:1]), reads=[src], writes=[junk, ag])
    P.op('act', lambda e: e.activation(out=junk[:], in_=src[:], func=AF.Square, accum_out=ag[:, 1:2]), reads=[src], writes=[junk, ag])
    P.op('dve', lambda e: e.tensor_scalar(out=ag[:], in0=ag[:], scalar1=1.0 / D, scalar2=None, op0=ALU.mult), reads=[ag], writes=[ag])
    P.op('dve', lambda e: e.tensor_tensor(out=rstd[:], in0=ag[:, 0:1], in1=ag[:, 0:1], op=ALU.mult), reads=[ag], writes=[rstd])
    P.op('dve', lambda e: e.scalar_tensor_tensor(out=rstd[:], in0=ag[:, 1:2], scalar=EPS, in1=rstd[:], op0=ALU.add, op1=ALU.subtract),
         reads=[ag, rstd], writes=[rstd])
    P.op('act', lambda e: e.activation(out=rstd[:], in_=rstd[:], func=AF.Sqrt), reads=[rstd], writes=[rstd])
    P.op('dve', lambda e: e.reciprocal(out=rstd[:], in_=rstd[:]), reads=[rstd], writes=[rstd])
    P.op('dve', lambda e: e.tensor_scalar(out=tmp[:], in0=src[:], scalar1=ag[:, 0:1], scalar2=rstd[:, 0:1],
                                          op0=ALU.subtract, op1=ALU.mult), reads=[src, ag, rstd], writes=[tmp])
    P.op(PENG, lambda e: e.tensor_tensor(out=tmp[:], in0=tmp[:], in1=lnw[:], op=ALU.mult), reads=[tmp, lnw], writes=[tmp])
    P.op(PENG, lambda e: e.tensor_tensor(out=out_h[:], in0=tmp[:], in1=lnb[:], op=ALU.add), reads=[tmp, lnb], writes=[out_h])
    if out_u is not None:
        P.op('dve', lambda e: e.tensor_tensor(out=tmp[:], in0=out_h[:], in1=sc1[:], op=ALU.mult), reads=[out_h, sc1], writes=[tmp])
        P.op('dve', lambda e: e.tensor_tensor(out=out_u[:], in0=tmp[:], in1=sh[:], op=ALU.add), reads=[tmp, sh], writes=[out_u])


def _bc(v):
    return np.ascontiguousarray(np.broadcast_to(v.reshape(1, -1), (128, v.size))).astype(np.float32)


def launch_tokA(h_cores, partials, prev, cur, NT):
    P = Prog()
    R = NT * 128
    h_d = P.din("h", [R, D])
    ident_d = P.din("ident", [128, 128])
    ident = P.sb("ident_s", [128, 128])
    P.dma('sp', ident, ident[:], ident_d, ident_d[:, :])
    names = []
    if prev is not None:
        names += ['m5_lat', 'm5_ctx', 'lnw', 'lnb']
        p_d = P.din("part", [8, R, D], BF16)
    if cur is not None:
        names += ['m0_lat', 'm1_lat', 'm0_ctx', 'm1_ctx']
        uT_d = P.dout("uT", [NT, 128, D], BF16)
    if prev is not None:
        hn_d = P.dout("hn", [R, D])
    mod_d = P.din("modb", [128, max(1, len(names)) * D])
    mb = {}
    for i, nm in enumerate(names):
        mb[nm] = P.sb("mb_" + nm, [128, D])
        P.dma('sp', mb[nm], mb[nm][:], mod_d, mod_d[:, i * D:(i + 1) * D])
    if cur is not None:
        for nm in ('m1_lat', 'm1_ctx'):
            P.op('dve', lambda e, nm=nm: e.tensor_scalar(out=mb[nm][:], in0=mb[nm][:], scalar1=1.0, scalar2=None, op0=ALU.add),
                 reads=[mb[nm]], writes=[mb[nm]])
    W = dict(junk=P.sb("junk", [128, D]), ag=P.sb("ag", [128, 2]), rstd=P.sb("rstd", [128, 1]), tmp=P.sb("tmp", [128, D]))
    hb = [P.sb("hb%d" % i, [128, D]) for i in range(2)]
    pb = [P.sb("pb%d" % i, [128, 8 * D], BF16) for i in range(2)] if prev is not None else None
    acc = P.sb("acc", [128, D])
    acc2 = P.sb("acc2", [128, D])
    hn = [P.sb("hn%d" % i, [128, D]) for i in range(2)]
    ub = [P.sb("ub%d" % i, [128, D]) for i in range(2)]
    utb = [P.sb("utb%d" % i, [128, D], BF16) for i in range(2)]
    ptr = [P.ps("ptr%d" % i, [128, D]) for i in range(2)]
    for t in range(NT):
        sfx = 'ctx' if t < CTX // 128 else 'lat'
        h = hb[t % 2]
        P.dma('sp', h, h[:], h_d, h_d[t * 128:(t + 1) * 128, :])
        if prev is not None:
            pt = pb[t % 2]
            P.dma('sp', pt, pt[:].rearrange("p (r d) -> p r d", r=8), p_d,
                  p_d[:, t * 128:(t + 1) * 128, :].rearrange("r p d -> p r d"))
            P.op('dve', lambda e, pt=pt: e.tensor_tensor(out=acc[:], in0=pt[:, 0:D], in1=pt[:, D:2 * D], op=ALU.add), reads=[pt], writes=[acc])
            P.op(PENG, lambda e, pt=pt: e.tensor_tensor(out=acc2[:], in0=pt[:, 4 * D:5 * D], in1=pt[:, 5 * D:6 * D], op=ALU.add), reads=[pt], writes=[acc2])
            for r in (2, 3):
                P.op('dve', lambda e, pt=pt, r=r: e.tensor_tensor(out=acc[:], in0=acc[:], in1=pt[:, r * D:(r + 1) * D], op=ALU.add), reads=[pt, acc], writes=[acc])
            for r in (6, 7):
                P.op(PENG, lambda e, pt=pt, r=r: e.tensor_tensor(out=acc2[:], in0=acc2[:], in1=pt[:, r * D:(r + 1) * D], op=ALU.add), reads=[pt, acc2], writes=[acc2])
            P.op('dve', lambda e: e.tensor_tensor(out=acc[:], in0=acc[:], in1=acc2[:], op=ALU.add), reads=[acc, acc2], writes=[acc])
            m5 = mb['m5_' + sfx]
            P.op('dve', lambda e, m5=m5: e.tensor_tensor(out=acc[:], in0=acc[:], in1=m5[:], op=ALU.mult), reads=[acc, m5], writes=[acc])
            P.op('dve', lambda e, h=h: e.scalar_tensor_tensor(out=acc[:], in0=h[:], scalar=ALPHA, in1=acc[:], op0=ALU.mult, op1=ALU.add),
                 reads=[h, acc], writes=[acc])
            hnew = hn[t % 2]
            u = ub[t % 2] if cur is not None else None
            _ln_mod(P, W, acc, mb['lnw'], mb['lnb'], mb.get('m1_' + sfx), mb.get('m0_' + sfx), hnew, u)
            P.dma('sp', hn_d, hn_d[t * 128:(t + 1) * 128, :], hnew, hnew[:], disjoint=True)
        else:
            u = ub[t % 2]
            tmp = W['tmp']
            P.op('dve', lambda e, h=h, sfx=sfx: e.tensor_tensor(out=tmp[:], in0=h[:], in1=mb['m1_' + sfx][:], op=ALU.mult), reads=[h, mb['m1_' + sfx]], writes=[tmp])
            P.op('dve', lambda e, u=u, sfx=sfx: e.tensor_tensor(out=u[:], in0=tmp[:], in1=mb['m0_' + sfx][:], op=ALU.add), reads=[tmp, mb['m0_' + sfx]], writes=[u])
        if cur is not None:
            pz = ptr[t % 2]
            for k in range(KC):
                P.tr(pz, pz[:, k * 128:(k + 1) * 128], u, u[:, k * 128:(k + 1) * 128], ident, ident[:])
            ut = utb[t % 2]
            P.op('act', lambda e, ut=ut, pz=pz: e.activation(out=ut[:], in_=pz[:], func=AF.Copy), reads=[pz], writes=[ut])
            P.dma('sp', uT_d, uT_d[t, :, :], ut, ut[:], disjoint=True)
    in_maps = []
    for c in range(NCORES):
        m = {"h": np.ascontiguousarray(h_cores[c], dtype=np.float32), "ident": np.eye(128, dtype=np.float32)}
        src = {}
        if prev is not None:
            src.update(prev)
            m["part"] = np.ascontiguousarray(partials[c])
        if cur is not None:
            src.update(cur)
        m["modb"] = np.concatenate([_bc(src[nm]) for nm in names], 1) if names else np.zeros((128, D), np.float32)
        in_maps.append(m)
    res = P.run(in_maps)
    h_new = [res[c]["hn"] for c in range(NCORES)] if prev is not None else h_cores
    uT = [res[c]["uT"] for c in range(NCORES)] if cur is not None else None
    return h_new, uT


def launch_tokB(h_cores, oT_cores, wout, mod, rw, rb, NT, HK):
    P = Prog()
    R = NT * 128
    names = ['m2_lat', 'm2_ctx', 'lnw', 'lnb', 'm3_lat', 'm4_lat', 'm3_ctx', 'm4_ctx']
    h_d = P.din("h", [R, D])
    oT_d = P.din("oT", [NT, 128, HK * 128], BF16)
    w_d = P.din("wout", [HK, 128, D])
    mod_d = P.din("modb", [128, len(names) * D])
    rw_d = P.din("rw", [128, KC * NEXP])
    rb_d = P.din("rb", [1, NEXP])
    ident_d = P.din("ident", [128, 128])
    h1_d = P.dout("h1", [R, D])
    vT_d = P.dout("vT", [NT, 128, D], BF16)
    g_d = P.dout("gates", [R, NEXP])
    ident = P.sb("ident_s", [128, 128])
    P.dma('sp', ident, ident[:], ident_d, ident_d[:, :])
    rws = P.sb("rw_s", [128, KC * NEXP])
    P.dma('sp', rws, rws[:], rw_d, rw_d[:, :])
    rbs = P.sb("rb_s", [1, NEXP])
    P.dma('sp', rbs, rbs[:], rb_d, rb_d[:, :])
    ones = P.sb("ones", [1, 128])
    P.op('dve', lambda e: e.memset(ones[:], 1.0), writes=[ones])
    mb = {}
    for i, nm in enumerate(names):
        mb[nm] = P.sb("mb_" + nm, [128, D])
        P.dma('sp', mb[nm], mb[nm][:], mod_d, mod_d[:, i * D:(i + 1) * D])
    for nm in ('m4_lat', 'm4_ctx'):
        P.op('dve', lambda e, nm=nm: e.tensor_scalar(out=mb[nm][:], in0=mb[nm][:], scalar1=1.0, scalar2=None, op0=ALU.add),
             reads=[mb[nm]], writes=[mb[nm]])
    wob = P.sb("wob", [128, HK * D], BF16)
    wst = [P.sb("wst%d" % i, [128, D]) for i in range(2)]
    for k in range(HK):
        ws = wst[k % 2]
        P.dma('sp', ws, ws[:], w_d, w_d[k, :, :])
        eng = 'act' if (k % 2 == 0 or DBG.get('nopoolcast')) else 'pool'
        if eng == 'act':
            P.op('act', lambda e, ws=ws, k=k: e.activation(out=wob[:, k * D:(k + 1) * D], in_=ws[:], func=AF.Copy), reads=[ws], writes=[wob])
        else:
            P.op('pool', lambda e, ws=ws, k=k: e.tensor_copy(out=wob[:, k * D:(k + 1) * D], in_=ws[:]), reads=[ws], writes=[wob])
    W = dict(junk=P.sb("junk", [128, D]), ag=P.sb("ag", [128, 2]), rstd=P.sb("rstd", [128, 1]), tmp=P.sb("tmp", [128, D]))
    hb = [P.sb("hb%d" % i, [128, D]) for i in range(2)]
    ob = [P.sb("ob%d" % i, [128, HK * 128], BF16) for i in range(2)]
    acc = P.sb("acc", [128, D])
    hn = [P.sb("hn%d" % i, [128, D]) for i in range(2)]
    vb = [P.sb("vb%d" % i, [128, D]) for i in range(2)]
    vt32 = P.sb("vt32", [128, D])
    vtb = [P.sb("vtb%d" % i, [128, D], BF16) for i in range(2)]
    lg = P.sb("lg", [128, NEXP])
    m8 = P.sb("m8", [128, 8])
    negm = P.sb("negm", [128, 1])
    msk = P.sb("msk", [128, NEXP])
    ex = P.sb("ex", [128, NEXP])
    ssum = P.sb("ssum", [128, 1])
    gt = [P.sb("gt%d" % i, [128, NEXP]) for i in range(2)]
    py = [P.ps("py%d" % i, [128, 512]) for i in range(2)]
    ptr = P.ps("ptr", [128, D])
    pl = P.ps("pl", [128, NEXP])
    for t in range(NT):
        sfx = 'ctx' if t < CTX // 128 else 'lat'
        h = hb[t % 2]
        o = ob[t % 2]
        if DBG.get('cut', 9) < 1:
            continue
        P.dma('sp', h, h[:], h_d, h_d[t * 128:(t + 1) * 128, :])
        P.dma('sp', o, o[:], oT_d, oT_d[t, :, :])
        if DBG.get('cut', 9) < 2:
            continue
        for nh in range(2):
            for k in range(HK):
                P.mm(py[nh], py[nh][:, :], o, o[:, k * 128:(k + 1) * 128], wob, wob[:, k * D + nh * 512:k * D + (nh + 1) * 512],
                     k == 0, k == HK - 1)
        if DBG.get('cut', 9) < 3:
            continue
        m2 = mb['m2_' + sfx]
        for nh in range(2):
            P.op('dve', lambda e, nh=nh, m2=m2: e.tensor_tensor(out=acc[:, nh * 512:(nh + 1) * 512], in0=py[nh][:, :],
                                                                in1=m2[:, nh * 512:(nh + 1) * 512], op=ALU.mult),
                 reads=[py[nh], m2], writes=[acc])
        P.op('dve', lambda e, h=h: e.scalar_tensor_tensor(out=acc[:], in0=h[:], scalar=ALPHA, in1=acc[:], op0=ALU.mult, op1=ALU.add),
             reads=[h, acc], writes=[acc])
        if DBG.get('cut', 9) < 4:
            continue
        h1 = hn[t % 2]
        v = vb[t % 2]
        _ln_mod(P, W, acc, mb['lnw'], mb['lnb'], mb['m4_' + sfx], mb['m3_' + sfx], h1, v)
        P.dma('sp', h1_d, h1_d[t * 128:(t + 1) * 128, :], h1, h1[:], disjoint=True)
        if DBG.get('cut', 9) < 5:
            continue
        for k in range(KC):
            P.tr(ptr, ptr[:, k * 128:(k + 1) * 128], v, v[:, k * 128:(k + 1) * 128], ident, ident[:])
        P.op('act', lambda e: e.activation(out=vt32[:], in_=ptr[:], func=AF.Copy), reads=[ptr], writes=[vt32])
        vt = vtb[t % 2]
        P.op('dve', lambda e, vt=vt: e.tensor_copy(out=vt[:], in_=vt32[:]), reads=[vt32], writes=[vt])
        P.dma('sp', vT_d, vT_d[t, :, :], vt, vt[:], disjoint=True)
        if DBG.get('norouter'):
            continue
        for k in range(KC):
            P.mm(pl, pl[:, :], vt32, vt32[:, k * 128:(k + 1) * 128], rws, rws[:, k * NEXP:(k + 1) * NEXP], k == 0, False)
        P.mm(pl, pl[:, :], ones, ones[:, :], rbs, rbs[:, :], False, True)
        P.op('dve', lambda e: e.tensor_copy(out=lg[:], in_=pl[:]), reads=[pl], writes=[lg])
        P.op('dve', lambda e: e.max(out=m8[:], in_=lg[:]), reads=[lg], writes=[m8])
        P.op('dve', lambda e: e.tensor_scalar(out=msk[:], in0=lg[:], scalar1=m8[:, 3:4], scalar2=None, op0=ALU.is_ge),
             reads=[lg, m8], writes=[msk])
        P.op('dve', lambda e: e.tensor_scalar(out=negm[:], in0=m8[:, 0:1], scalar1=-1.0, scalar2=None, op0=ALU.mult),
             reads=[m8], writes=[negm])
        P.op('act', lambda e: e.activation(out=ex[:], in_=lg[:], func=AF.Exp, bias=negm[:, 0:1], scale=1.0),
             reads=[lg, negm], writes=[ex])
        P.op('dve', lambda e: e.tensor_tensor(out=ex[:], in0=ex[:], in1=msk[:], op=ALU.mult), reads=[ex, msk], writes=[ex])
        P.op('dve', lambda e: e.reduce_sum(out=ssum[:], in_=ex[:], axis=AX.X), reads=[ex], writes=[ssum])
        P.op('dve', lambda e: e.reciprocal(out=ssum[:], in_=ssum[:]), reads=[ssum], writes=[ssum])
        g = gt[t % 2]
        P.op('dve', lambda e, g=g: e.tensor_scalar(out=g[:], in0=ex[:], scalar1=ssum[:, 0:1], scalar2=None, op0=ALU.mult),
             reads=[ex, ssum], writes=[g])
        P.dma('sp', g_d, g_d[t * 128:(t + 1) * 128, :], g, g[:], disjoint=True)
    rw_np = np.ascontiguousarray(rw.reshape(KC, 128, NEXP).transpose(1, 0, 2).reshape(128, KC * NEXP))
    modb = np.concatenate([_bc(mod[nm]) for nm in names], 1)
    w_np = np.ascontiguousarray(wout.reshape(HK, 128, D), dtype=np.float32)
    in_maps = []
    for c in range(NCORES):
        in_maps.append({"h": np.ascontiguousarray(h_cores[c], dtype=np.float32), "oT": np.ascontiguousarray(oT_cores[c]),
                        "wout": w_np, "modb": modb, "rw": rw_np, "rb": np.ascontiguousarray(rb.reshape(1, NEXP)),
                        "ident": np.eye(128, dtype=np.float32)})
    res = P.run(in_maps)
    return ([res[c]["h1"] for c in range(NCORES)], [res[c]["vT"] for c in range(NCORES)],
            [res[c]["gates"] for c in range(NCORES)])


SWL = 7.0
SWA = 1.702


def launch_moe(vT_all, gates_all, wgu, bgu_in, wdn, bdn, TT):
    P = Prog()
    NTT = TT // 128
    vT_d = P.din("vT", [D, TT], BF16)
    g_d = P.din("g", [TT, EPC])
    gT_d = P.din("gT", [EPC, TT])
    wgu_d = P.din("wgu", [EPC * KC, 128, 2 * D])
    wdn_d = P.din("wdn", [EPC * KC, 128, D])
    bgu_d = P.din("bgu", [128, EPC * 16])
    bdn_d = P.din("bdn", [EPC, D])
    out_d = P.dout("part", [TT, D], BF16)
    wgu_s = P.dscratch("wgu_b", [EPC * KC, 128, 2 * D], BF16)
    wdn_s = P.dscratch("wdn_b", [EPC * KC, 128, D], BF16)
    bgu = P.sb("bgu_s", [128, EPC * 16])
    P.dma('sp', bgu, bgu[:], bgu_d, bgu_d[:, :])
    bdn_t = P.sb("bdn_s", [EPC, D])
    P.dma('sp', bdn_t, bdn_t[:], bdn_d, bdn_d[:, :])
    st32 = [P.sb("st32_%d" % i, [128, D]) for i in range(2)]
    st16 = [P.sb("st16_%d" % i, [128, D], BF16) for i in range(2)]
    n = 0
    for i in range(EPC * KC):
        for (src_d, dst_s, c0) in ((wgu_d, wgu_s, 0), (wgu_d, wgu_s, D), (wdn_d, wdn_s, 0)):
            a, b = st32[n % 2], st16[n % 2]
            P.dma('sp', a, a[:, :], src_d, src_d[i, :, c0:c0 + D])
            if n % 2 == 0:
                P.op('act', lambda e, a=a, b=b: e.activation(out=b[:, :], in_=a[:, :], func=AF.Copy), reads=[a], writes=[b])
            else:
                P.op('dve', lambda e, a=a, b=b: e.tensor_copy(out=b[:, :], in_=a[:, :]), reads=[a], writes=[b])
            P.dma('sp', dst_s, dst_s[i, :, c0:c0 + D], b, b[:, :], disjoint=True)
            n += 1
    SBT = 1024
    wg = [P.sb("wg%d" % i, [128, KC * 2 * D], BF16) for i in range(2)]
    wd = [P.sb("wd%d" % i, [128, KC * D], BF16) for i in range(2)]
    vts = P.sb("vts", [128, KC * SBT], BF16)
    acc = [P.sb("acc%d" % i, [128, D]) for i in range(SBT // 128)]
    gsb = P.sb("gsb", [128, (SBT // 128) * EPC])
    accb = [P.sb("accb%d" % i, [128, D], BF16) for i in range(2)]
    gTs = P.sb("gTs", [EPC, SBT])
    actT = [P.sb("actT%d" % i, [128, KC * 512], BF16) for i in range(2)]
    g1 = [P.sb("g1_%d" % i, [128, 512]) for i in range(2)]
    sg = [P.sb("sg_%d" % i, [128, 512]) for i in range(2)]
    u1 = [P.sb("u1_%d" % i, [128, 512]) for i in range(2)]
    u2 = u1
    gl = sg
    pg = [P.ps("pg%d" % i, [128, 512]) for i in range(2)]
    pu = [P.ps("pu%d" % i, [128, 512]) for i in range(2)]
    pd = [P.ps("pd%d" % i, [128, 512]) for i in range(4)]
    wn = 0
    cn = 0
    dn = 0
    for s0 in range(0, TT, SBT):
        sl = min(SBT, TT - s0)
        nt_sb = sl // 128
        P.dma('sp', vts, vts[:, 0:KC * sl].rearrange("p (k t) -> p k t", k=KC), vT_d,
              vT_d[:, s0:s0 + sl].rearrange("(k p) t -> p k t", p=128))
        P.dma('sp', gsb, gsb[:, 0:nt_sb * EPC].rearrange("p (t e) -> p t e", e=EPC), g_d,
              g_d[s0:s0 + sl, :].rearrange("(t p) e -> p t e", p=128))
        P.dma('sp', gTs, gTs[:, 0:sl], gT_d, gT_d[:, s0:s0 + sl])
        for ti in range(nt_sb):
            for nh in range(2):
                pt = pd[dn % 4]
                dn += 1
                P.mm(pt, pt[:, :], gTs, gTs[:, ti * 128:(ti + 1) * 128], bdn_t, bdn_t[:, nh * 512:(nh + 1) * 512], True, True)
                P.op('act', lambda e, ti=ti, nh=nh, pt=pt: e.activation(out=acc[ti][:, nh * 512:(nh + 1) * 512], in_=pt[:, :], func=AF.Copy),
                     reads=[pt], writes=[acc[ti]])
        for e_ in range(EPC):
            wgt, wdt = wg[wn % 2], wd[wn % 2]
            wn += 1
            P.dma('sp', wgt, wgt[:].rearrange("p (k n) -> p k n", k=KC), wgu_s,
                  wgu_s[e_ * KC:(e_ + 1) * KC, :, :].rearrange("k p n -> p k n"))
            P.dma('sp', wdt, wdt[:].rearrange("p (k n) -> p k n", k=KC), wdn_s,
                  wdn_s[e_ * KC:(e_ + 1) * KC, :, :].rearrange("k p n -> p k n"))
            for b0 in range(0, sl, 512):
                nb = min(512, sl - b0)
                at = actT[cn % 2]
                for j in range(KC):
                    q = cn % 2
                    cn += 1
                    for k in range(KC):
                        P.mm(pg[q], pg[q][:, 0:nb], wgt, wgt[:, k * 2 * D + j * 128:k * 2 * D + (j + 1) * 128],
                             vts, vts[:, k * sl + b0:k * sl + b0 + nb], k == 0, k == KC - 1)
                    for k in range(KC):
                        P.mm(pu[q], pu[q][:, 0:nb], wgt, wgt[:, k * 2 * D + D + j * 128:k * 2 * D + D + (j + 1) * 128],
                             vts, vts[:, k * sl + b0:k * sl + b0 + nb], k == 0, k == KC - 1)
                    cg = e_ * 16 + j
                    cu = e_ * 16 + 8 + j
                    P.op('dve', lambda e, q=q, nb=nb, cg=cg: e.tensor_scalar(out=g1[q][:, 0:nb], in0=pg[q][:, 0:nb], scalar1=bgu[:, cg:cg + 1],
                                                                          scalar2=SWL, op0=ALU.add, op1=ALU.min), reads=[pg[q], bgu], writes=[g1[q]])
                    P.op('act', lambda e, q=q, nb=nb: e.activation(out=sg[q][:, 0:nb], in_=g1[q][:, 0:nb], func=AF.Sigmoid, scale=SWA),
                         reads=[g1[q]], writes=[sg[q]])
                    P.op('dve', lambda e, q=q, nb=nb, cu=cu: e.tensor_scalar(out=u1[q][:, 0:nb], in0=pu[q][:, 0:nb], scalar1=bgu[:, cu:cu + 1],
                                                                          scalar2=SWL, op0=ALU.add, op1=ALU.min), reads=[pu[q], bgu], writes=[u1[q]])
                    P.op('pool', lambda e, q=q, nb=nb: e.tensor_scalar(out=u2[q][:, 0:nb], in0=u1[q][:, 0:nb], scalar1=-SWL, scalar2=1.0,
                                                                       op0=ALU.max, op1=ALU.add), reads=[u1[q]], writes=[u2[q]])
                    P.op('pool', lambda e, q=q, nb=nb: e.tensor_tensor(out=gl[q][:, 0:nb], in0=g1[q][:, 0:nb], in1=sg[q][:, 0:nb], op=ALU.mult),
                         reads=[g1[q], sg[q]], writes=[gl[q]])
                    P.op('dve', lambda e, q=q, nb=nb, j=j, at=at: e.tensor_tensor(out=at[:, j * 512:j * 512 + nb], in0=gl[q][:, 0:nb], in1=u2[q][:, 0:nb], op=ALU.mult),
                         reads=[gl[q], u2[q]], writes=[at])
                for ti in range(nb // 128):
                    tg = (b0 // 128) + ti
                    for nh in range(2):
                        pt = pd[dn % 4]
                        dn += 1
                        for j in range(KC):
                            P.mm(pt, pt[:, :], at, at[:, j * 512 + ti * 128:j * 512 + (ti + 1) * 128], wdt,
                                 wdt[:, j * D + nh * 512:j * D + (nh + 1) * 512], j == 0, j == KC - 1)
                        gc = tg * EPC + e_
                        P.op('dve', lambda e, pt=pt, tg=tg, nh=nh, gc=gc: e.scalar_tensor_tensor(
                            out=acc[tg][:, nh * 512:(nh + 1) * 512], in0=pt[:, :], scalar=gsb[:, gc:gc + 1],
                            in1=acc[tg][:, nh * 512:(nh + 1) * 512], op0=ALU.mult, op1=ALU.add), reads=[pt, gsb, acc[tg]], writes=[acc[tg]])
        for ti in range(nt_sb):
            ab_ = accb[ti % 2]
            P.op('act', lambda e, ab_=ab_, ti=ti: e.activation(out=ab_[:], in_=acc[ti][:], func=AF.Copy), reads=[acc[ti]], writes=[ab_])
            P.dma('sp', out_d, out_d[s0 + ti * 128:s0 + (ti + 1) * 128, :], ab_, ab_[:], disjoint=True)
    in_maps = []
    for c in range(NCORES):
        es = slice(c * EPC, (c + 1) * EPC)
        in_maps.append({
            "vT": np.ascontiguousarray(vT_all), "g": np.ascontiguousarray(gates_all[:, es]),
            "gT": np.ascontiguousarray(gates_all[:, es].T),
            "wgu": np.ascontiguousarray(wgu[es].reshape(EPC * KC, 128, 2 * D)),
            "wdn": np.ascontiguousarray(wdn[es].reshape(EPC * KC, 128, D)),
            "bgu": np.ascontiguousarray(bgu_np_layout(bgu_in[es])),
            "bdn": np.ascontiguousarray(bdn[es])})
    res = P.run(in_maps)
    return [res[c]["part"] for c in range(NCORES)]


def bgu_np(bgu, es):
    return bgu[es]


def bgu_np_layout(bgu_all):
    return bgu_all.reshape(EPC, 16, 128).transpose(2, 0, 1).reshape(128, EPC * 16)


def rope_tables(L_lat):
    GRID_W = 64
    pos = np.arange(L_lat)
    row = (pos // GRID_W).astype(np.float32)
    col = (pos % GRID_W).astype(np.float32)
    axis_dim = 32
    inv = (10000.0 ** (-np.arange(0, axis_dim, 2, dtype=np.float32) / axis_dim)).astype(np.float32)
    ar = row[:, None] * inv
    ac = col[:, None] * inv
    ang = np.concatenate([ar, ar, ac, ac], -1)
    cos = np.cos(ang).astype(np.float32)
    sin = np.sin(ang).astype(np.float32)
    sign = np.concatenate([-np.ones(16), np.ones(16), -np.ones(16), np.ones(16)]).astype(np.float32)
    cosT = np.concatenate([np.ones((64, CTX), np.float32), cos.T], 1)
    sinT = np.concatenate([np.zeros((64, CTX), np.float32), (sin * sign).T], 1)
    return np.ascontiguousarray(cosT), np.ascontiguousarray(sinT)


ROT_PERM = np.concatenate([np.arange(16, 32), np.arange(0, 16), np.arange(48, 64), np.arange(32, 48)])


def launch_attn(uT_all, w_in, lam, subln_w, lambda_init, TT):
    P = Prog()
    NTT = TT // 128
    L = TT - CTX
    uT_d = P.din("uT", [D, TT], BF16)
    w_d = P.din("w", [5 * KC, 128, 128])
    cos_d = P.din("cos", [64, TT])
    sin_d = P.din("sin", [64, TT])
    lam_d = P.din("lam", [128, 256])
    sw_d = P.din("sw", [128, 1])
    sel_d = P.din("sel", [128, 256], BF16)
    oT_d = P.dout("oT", [128, TT], BF16)
    sel = P.sb("sel_s", [128, 256], BF16)
    P.dma('sp', sel, sel[:], sel_d, sel_d[:, :])
    ones_b = P.sb("ones_b", [128, 128], BF16)
    ones_f = P.sb("ones_f", [128, 128])
    P.op('dve', lambda e: e.memset(ones_b[:], 1.0), writes=[ones_b])
    P.op('dve', lambda e: e.memset(ones_f[:], 1.0), writes=[ones_f])
    lamt = P.sb("lamt", [128, 256])
    P.dma('sp', lamt, lamt[:], lam_d, lam_d[:, :])
    sw = P.sb("sw_s", [128, 1])
    P.dma('sp', sw, sw[:], sw_d, sw_d[:, :])
    P.op('dve', lambda e: e.tensor_scalar(out=sw[:], in0=sw[:], scalar1=1.0 - lambda_init, scalar2=None, op0=ALU.mult), reads=[sw], writes=[sw])
    lp = P.sb("lp", [128, 128])
    ls = P.sb("ls", [128, 2])
    neglam = P.sb("neglam", [128, 1])
    P.op('dve', lambda e: e.tensor_tensor(out=lp[:, 0:64], in0=lamt[:, 0:64], in1=lamt[:, 64:128], op=ALU.mult), reads=[lamt], writes=[lp])
    P.op('dve', lambda e: e.tensor_tensor(out=lp[:, 64:128], in0=lamt[:, 128:192], in1=lamt[:, 192:256], op=ALU.mult), reads=[lamt], writes=[lp])
    P.op('dve', lambda e: e.reduce_sum(out=ls[:, 0:1], in_=lp[:, 0:64], axis=AX.X), reads=[lp], writes=[ls])
    P.op('dve', lambda e: e.reduce_sum(out=ls[:, 1:2], in_=lp[:, 64:128], axis=AX.X), reads=[lp, ls], writes=[ls])
    P.op('act', lambda e: e.activation(out=ls[:], in_=ls[:], func=AF.Exp), reads=[ls], writes=[ls])
    P.op('dve', lambda e: e.tensor_tensor(out=neglam[:], in0=ls[:, 1:2], in1=ls[:, 0:1], op=ALU.subtract), reads=[ls], writes=[neglam])
    P.op('dve', lambda e: e.tensor_scalar(out=neglam[:], in0=neglam[:], scalar1=-lambda_init, scalar2=None, op0=ALU.add), reads=[neglam], writes=[neglam])
    wst = [P.sb("wst%d" % i, [128, 128]) for i in range(2)]
    wb = P.sb("wb", [128, 5 * KC * 128], BF16)
    for i in range(5 * KC):
        a = wst[i % 2]
        P.dma('sp', a, a[:], w_d, w_d[i, :, :])
        P.op('act', lambda e, a=a, i=i: e.activation(out=wb[:, i * 128:(i + 1) * 128], in_=a[:], func=AF.Copy), reads=[a], writes=[wb])
    QT = P.sb("QT", [128, TT], BF16)
    KT = P.sb("KT", [128, TT], BF16)
    Vs = P.sb("Vs", [128, NTT * 128], BF16)
    ub = [P.sb("ub%d" % i, [128, KC * 512], BF16) for i in range(2)]
    cs = [P.sb("cs%d" % i, [128, 512]) for i in range(2)]
    sn = [P.sb("sn%d" % i, [128, 512]) for i in range(2)]
    x1 = P.sb("x1", [128, 512])
    x2 = P.sb("x2", [128, 512])
    sq = P.sb("sq", [128, 512], BF16)
    mx = P.sb("mx", [128, 4])
    mt = P.sb("mt", [128, 1])
    P.op('dve', lambda e: e.memset(mx[:], 0.0), writes=[mx])
    pA = [P.ps("pA%d" % i, [128, 512]) for i in range(2)]
    pO = [P.ps("pO%d" % i, [128, 512]) for i in range(2)]
    pL = [P.ps("pL%d" % i, [128, 512]) for i in range(2)]
    pN = P.ps("pN", [128, 512])
    for bi, b0 in enumerate(range(0, TT, 512)):
        nb = min(512, TT - b0)
        u = ub[bi % 2]
        c_, s_ = cs[bi % 2], sn[bi % 2]
        P.dma('sp', u, u[:, 0:KC * nb].rearrange("p (k t) -> p k t", k=KC), uT_d, uT_d[:, b0:b0 + nb].rearrange("(k p) t -> p k t", p=128))
        for hh in range(2):
            P.dma('sp', c_, c_[hh * 64:(hh + 1) * 64, 0:nb], cos_d, cos_d[:, b0:b0 + nb])
            P.dma('sp', s_, s_[hh * 64:(hh + 1) * 64, 0:nb], sin_d, sin_d[:, b0:b0 + nb])
        for xi, XT in enumerate((QT, KT)):
            for k in range(KC):
                P.mm(pA[0], pA[0][:, 0:nb], wb, wb[:, (2 * xi * KC + k) * 128:(2 * xi * KC + k + 1) * 128], u, u[:, k * nb:(k + 1) * nb], k == 0, k == KC - 1)
            for k in range(KC):
                P.mm(pA[1], pA[1][:, 0:nb], wb, wb[:, ((2 * xi + 1) * KC + k) * 128:((2 * xi + 1) * KC + k + 1) * 128], u, u[:, k * nb:(k + 1) * nb], k == 0, k == KC - 1)
            P.op('dve', lambda e, nb=nb, c_=c_: e.tensor_tensor(out=x1[:, 0:nb], in0=pA[0][:, 0:nb], in1=c_[:, 0:nb], op=ALU.mult), reads=[pA[0], c_], writes=[x1])
            P.op('dve', lambda e, nb=nb, s_=s_: e.tensor_tensor(out=x2[:, 0:nb], in0=pA[1][:, 0:nb], in1=s_[:, 0:nb], op=ALU.mult), reads=[pA[1], s_], writes=[x2])
            P.op('dve', lambda e, nb=nb, XT=XT, b0=b0: e.tensor_tensor(out=XT[:, b0:b0 + nb], in0=x1[:, 0:nb], in1=x2[:, 0:nb], op=ALU.add), reads=[x1, x2], writes=[XT])
            P.op('act', lambda e, nb=nb, XT=XT, b0=b0: e.activation(out=sq[:, 0:nb], in_=XT[:, b0:b0 + nb], func=AF.Square), reads=[XT], writes=[sq])
            for m in range(2):
                P.mm(pN, pN[:, 0:nb], sel, sel[:, m * 128:(m + 1) * 128], sq, sq[:, 0:nb], True, True)
                P.op('dve', lambda e, nb=nb: e.reduce_max(out=mt[:], in_=pN[:, 0:nb], axis=AX.X), reads=[pN], writes=[mt])
                col = xi * 2 + m
                P.op('dve', lambda e, col=col: e.tensor_tensor(out=mx[:, col:col + 1], in0=mx[:, col:col + 1], in1=mt[:], op=ALU.max), reads=[mx, mt], writes=[mx])
        for ti in range(nb // 128):
            tg = b0 // 128 + ti
            pv = pO[ti % 2]
            for k in range(KC):
                P.mm(pv, pv[:, 0:128], u, u[:, k * nb + ti * 128:k * nb + (ti + 1) * 128], wb, wb[:, (4 * KC + k) * 128:(4 * KC + k + 1) * 128], k == 0, k == KC - 1)
            P.op('act', lambda e, pv=pv, tg=tg: e.activation(out=Vs[:, tg * 128:(tg + 1) * 128], in_=pv[:, 0:128], func=AF.Copy), reads=[pv], writes=[Vs])
    negc = P.sb("negc", [128, 2])
    P.op('dve', lambda e: e.tensor_tensor(out=negc[:], in0=mx[:, 0:2], in1=mx[:, 2:4], op=ALU.mult), reads=[mx], writes=[negc])
    P.op('act', lambda e: e.activation(out=negc[:], in_=negc[:], func=AF.Sqrt), reads=[negc], writes=[negc])
    P.op('dve', lambda e: e.tensor_scalar(out=negc[:], in0=negc[:], scalar1=-1.05 * 0.125, scalar2=None, op0=ALU.mult), reads=[negc], writes=[negc])
    Pt = [P.sb("Pt%d" % i, [128, 512], BF16) for i in range(3)]
    r0 = P.sb("r0", [128, 512])
    r1 = P.sb("r1", [128, 512])
    o0 = P.sb("o0", [128, 512])
    o1 = P.sb("o1", [128, 512])
    a_ = P.sb("a_", [128, 512])
    sqf = P.sb("sqf", [128, 512])
    rs = P.sb("rs", [128, 512])
    ob = [P.sb("ob%d" % i, [128, 512], BF16) for i in range(2)]
    qblocks = [(0, CTX, CTX // 128)] + [(q0, min(512, TT - q0), NTT) for q0 in range(CTX, TT, 512)]
    it = 0
    for qi, (q0, nq, nkt) in enumerate(qblocks):
        for m in range(2):
            rows = slice(m * 64, (m + 1) * 64)
            for kt in range(nkt):
                ps = pA[it % 2]
                pt = Pt[it % 3]
                it += 1
                P.mm(ps, ps[:, 0:nq], KT, KT[rows, kt * 128:(kt + 1) * 128], QT, QT[rows, q0:q0 + nq], True, True)
                P.op('act', lambda e, ps=ps, pt=pt, nq=nq, m=m: e.activation(out=pt[:, 0:nq], in_=ps[:, 0:nq], func=AF.Exp, bias=negc[:, m:m + 1], scale=0.125),
                     reads=[ps, negc], writes=[pt])
                P.mm(pO[m], pO[m][:, 0:nq], Vs, Vs[:, kt * 128:(kt + 1) * 128], pt, pt[:, 0:nq], kt == 0, kt == nkt - 1)
                P.mm(pL[m], pL[m][:, 0:nq], ones_b, ones_b[:, :], pt, pt[:, 0:nq], kt == 0, kt == nkt - 1)
        P.op('dve', lambda e, nq=nq: e.reciprocal(out=r0[:, 0:nq], in_=pL[0][:, 0:nq]), reads=[pL[0]], writes=[r0])
        P.op('dve', lambda e, nq=nq: e.reciprocal(out=r1[:, 0:nq], in_=pL[1][:, 0:nq]), reads=[pL[1]], writes=[r1])
        P.op('dve', lambda e, nq=nq: e.tensor_tensor(out=o0[:, 0:nq], in0=pO[0][:, 0:nq], in1=r0[:, 0:nq], op=ALU.mult), reads=[pO[0], r0], writes=[o0])
        P.op('dve', lambda e, nq=nq: e.tensor_tensor(out=o1[:, 0:nq], in0=pO[1][:, 0:nq], in1=r1[:, 0:nq], op=ALU.mult), reads=[pO[1], r1], writes=[o1])
        P.op('dve', lambda e, nq=nq: e.scalar_tensor_tensor(out=a_[:, 0:nq], in0=o1[:, 0:nq], scalar=neglam[:, 0:1], in1=o0[:, 0:nq], op0=ALU.mult, op1=ALU.add),
             reads=[o0, o1, neglam], writes=[a_])
        P.op('act', lambda e, nq=nq: e.activation(out=sqf[:, 0:nq], in_=a_[:, 0:nq], func=AF.Square), reads=[a_], writes=[sqf])
        P.mm(pN, pN[:, 0:nq], ones_f, ones_f[:, :], sqf, sqf[:, 0:nq], True, True)
        P.op('act', lambda e, nq=nq: e.activation(out=rs[:, 0:nq], in_=pN[:, 0:nq], func=AF.Sqrt, bias=EPS, scale=1.0 / 128), reads=[pN], writes=[rs])
        P.op('dve', lambda e, nq=nq: e.reciprocal(out=rs[:, 0:nq], in_=rs[:, 0:nq]), reads=[rs], writes=[rs])
        P.op('dve', lambda e, nq=nq: e.tensor_tensor(out=a_[:, 0:nq], in0=a_[:, 0:nq], in1=rs[:, 0:nq], op=ALU.mult), reads=[a_, rs], writes=[a_])
        o_ = ob[qi % 2]
        P.op('dve', lambda e, nq=nq, o_=o_: e.tensor_scalar(out=o_[:, 0:nq], in0=a_[:, 0:nq], scalar1=sw[:, 0:1], scalar2=None, op0=ALU.mult), reads=[a_, sw], writes=[o_])
        P.dma('sp', oT_d, oT_d[:, q0:q0 + nq], o_, o_[:, 0:nq], disjoint=True)
    cosT, sinT = rope_tables(L)
    selnp = np.zeros((128, 256), np.float32)
    selnp[0:64, 0:128] = 1.0
    selnp[64:128, 128:256] = 1.0
    in_maps = []
    for c in range(NCORES):
        cols = []
        for base in (0, D):
            wc = w_in[:, base + c * 128:base + (c + 1) * 128]
            wp = np.concatenate([wc[:, 0:64][:, ROT_PERM], wc[:, 64:128][:, ROT_PERM]], 1)
            cols += [wc, wp]
        cols.append(w_in[:, 2 * D + c * 128:2 * D + (c + 1) * 128])
        wnp = np.stack([x.reshape(KC, 128, 128) for x in cols], 0).reshape(5 * KC, 128, 128)
        in_maps.append({"uT": np.ascontiguousarray(uT_all), "w": np.ascontiguousarray(wnp, dtype=np.float32),
                        "cos": cosT, "sin": sinT, "lam": _bc(lam.reshape(-1)), "sw": np.ascontiguousarray(subln_w.reshape(128, 1), dtype=np.float32),
                        "sel": selnp.astype(NPBF)})
    res = P.run(in_maps)
    return np.concatenate([res[c]["oT"] for c in range(NCORES)], 0)


def gdn_consts():
    i = np.arange(128)
    same = (i[:, None] // 64) == (i[None, :] // 64)
    le = i[:, None] <= i[None, :]
    ge = i[:, None] >= i[None, :]
    lt = i[:, None] < i[None, :]
    gt = i[:, None] > i[None, :]
    mats = []
    for d in range(2):
        mcum = same & (le if d == 0 else ge)
        maskS = same & (gt if d == 0 else lt)
        maskIT = same & (le if d == 0 else ge)
        mats += [mcum, maskS, maskIT]
    mats += [same, np.eye(128, dtype=bool)]
    c = np.concatenate([m.astype(np.float32) for m in mats], 1)
    cind = np.stack([(i < 64), (i >= 64)], 1).astype(np.float32)
    return np.ascontiguousarray(c), cind


def launch_gdn(uT_all, w_in, conv_w, a_log, dt_bias, norm_w, TT):
    P = Prog()
    NTT = TT // 128
    RSQ = 128 ** -0.5
    GW = 516
    uT_d = P.din("uT", [D, TT], BF16)
    w_d = P.din("w", [4 * KC, 128, 128])
    wz_d = P.din("wz", [KC, 128, 256])
    wab_d = P.din("wab", [KC, 128, 8])
    cw_d = P.din("cw", [128, 20])
    cst_d = P.din("cst", [128, 8 * 128])
    cind_d = P.din("cind", [128, 2])
    alog_d = P.din("alog", [128, 4])
    dtb_d = P.din("dtb", [128, 4])
    nw_d = P.din("nw", [128, 128])
    oT_d = P.dout("oT", [256, TT], BF16)
    of_s = P.dscratch("of_s", [TT, 256])
    dbg_d = P.dout("dbg", [TT, 1024]) if DBG.get("gdbg") else None

    def D_(fn, r, w):
        P.op('dve', fn, reads=r, writes=w)

    def A_(fn, r, w):
        P.op('act', fn, reads=r, writes=w)

    def G_(fn, r, w):
        P.op('pool', fn, reads=r, writes=w)

    def load(name, shape, src_d, dt=F32):
        b = P.sb(name, shape, dt)
        P.dma('sp', b, b[:], src_d, src_d[tuple(slice(None) for _ in shape)])
        return b
    cst = load("cst_s", [128, 8 * 128], cst_d)
    cind = load("cind_s", [128, 2], cind_d)
    cw = load("cw_s", [128, 20], cw_d)
    negA = load("negA", [128, 4], alog_d)
    dtb = load("dtb_s", [128, 4], dtb_d)
    nwb = load("nwb", [128, 128], nw_d)
    A_(lambda e: e.activation(out=negA[:], in_=negA[:], func=AF.Exp), [negA], [negA])
    D_(lambda e: e.tensor_scalar(out=negA[:], in0=negA[:], scalar1=-1.0, scalar2=None, op0=ALU.mult), [negA], [negA])

    def C(idx):
        return cst[:, idx * 128:(idx + 1) * 128]
    SAME, IDENT = 6, 7
    ones_f = P.sb("ones_f", [128, 128])
    negones = P.sb("negones", [128, 128])
    D_(lambda e: e.memset(ones_f[:], 1.0), [], [ones_f])
    D_(lambda e: e.memset(negones[:], -1.0), [], [negones])
    wst = [P.sb("wst%d" % i, [128, 256]) for i in range(2)]
    wb = P.sb("wb", [128, 4 * KC * 128], BF16)
    wzb = P.sb("wzb", [128, KC * 256], BF16)
    wabb = P.sb("wabb", [128, KC * 8], BF16)
    n = 0
    for i in range(4 * KC):
        a = wst[n % 2]; n += 1
        P.dma('sp', a, a[:, 0:128], w_d, w_d[i, :, :])
        A_(lambda e, a=a, i=i: e.activation(out=wb[:, i * 128:(i + 1) * 128], in_=a[:, 0:128], func=AF.Copy), [a], [wb])
    for k in range(KC):
        a = wst[n % 2]; n += 1
        P.dma('sp', a, a[:, 0:256], wz_d, wz_d[k, :, :])
        A_(lambda e, a=a, k=k: e.activation(out=wzb[:, k * 256:(k + 1) * 256], in_=a[:, 0:256], func=AF.Copy), [a], [wzb])
    for k in range(KC):
        a = wst[n % 2]; n += 1
        P.dma('sp', a, a[:, 0:8], wab_d, wab_d[k, :, :])
        A_(lambda e, a=a, k=k: e.activation(out=wabb[:, k * 8:(k + 1) * 8], in_=a[:, 0:8], func=AF.Copy), [a], [wabb])
    S = {}
    for d in range(2):
        for hl in range(2):
            S[(d, hl)] = P.sb("S%d%d" % (d, hl), [128, 128])
            D_(lambda e, b=S[(d, hl)]: e.memset(b[:], 0.0), [], [S[(d, hl)]])
    ug = [P.sb("ug%d" % i, [128, KC * GW], BF16) for i in range(2)]
    pA = [P.ps("pA%d" % i, [128, 512]) for i in range(2)]
    pD = P.ps("pD", [128, 512])
    pM = [P.ps("pM%d" % i, [128, 512]) for i in range(2)]
    pU = P.ps("pU", [128, 512])
    pR = P.ps("pR", [128, 512])
    pKV = P.ps("pKV", [128, 512]) if DBG.get("p8") else None
    pKVb = pKV if pKV is not None else pU
    kvo = 0 if pKV is not None else 256
    bufs = {}

    def B(name, par, shape=(128, 128), dt=F32):
        key = (name, par)
        if key not in bufs:
            bufs[key] = P.sb("%s_%d" % (name, par), list(shape), dt)
        return bufs[key]

    ctx_tiles = list(range(CTX // 128))
    lat_tiles = list(range(CTX // 128, NTT))

    def groups(tiles):
        return [tiles[i:i + 4] for i in range(0, len(tiles), 4)]
    order = {0: [(g, 0, CTX) for g in groups(ctx_tiles)] + [(g, CTX, TT) for g in groups(lat_tiles)],
             1: [(g[::-1], 0, CTX) for g in groups(ctx_tiles)[::-1]] + [(g[::-1], CTX, TT) for g in groups(lat_tiles)[::-1]]}
    gi = 0
    tcount = 0
    for d in range(2):
        MC, MS, MIT = 3 * d, 3 * d + 1, 3 * d + 2
        for (gt, seg0, seg1) in order[d]:
            g0 = min(gt) * 128
            g1 = (max(gt) + 1) * 128
            glo = max(g0 - 2, seg0)
            ghi = min(g1 + 2, seg1)
            gw = ghi - glo
            u = ug[gi % 2]
            gi += 1
            P.dma('sp', u, u[:].rearrange("p (k t) -> p k t", k=KC)[:, :, 0:gw], uT_d,
                  uT_d[:, glo:ghi].rearrange("(k p) t -> p k t", p=128))
            for tile in gt:
                par = tcount % 2
                tcount += 1
                t0 = tile * 128
                lo = max(t0 - 2, seg0)
                hi = min(t0 + 130, seg1)
                ncol = hi - lo
                off = lo - (t0 - 2)
                X = []
                for ci in range(4):
                    pp = pA[ci % 2]
                    for k in range(KC):
                        P.mm(pp, pp[:, 0:ncol], wb, wb[:, (ci * KC + k) * 128:(ci * KC + k + 1) * 128],
                             u, u[:, k * GW + lo - glo:k * GW + hi - glo], k == 0, k == KC - 1)
                    pb = B("pbuf%d" % ci, par, (128, 132))
                    D_(lambda e, pb=pb: e.memset(pb[:, 0:2], 0.0), [], [pb])
                    D_(lambda e, pb=pb: e.memset(pb[:, 130:132], 0.0), [], [pb])
                    A_(lambda e, pb=pb, pp=pp, off=off, ncol=ncol: e.activation(out=pb[:, off:off + ncol], in_=pp[:, 0:ncol], func=AF.Copy), [pp], [pb])
                    ca = B("cacc%d" % ci, par)
                    D_(lambda e, ca=ca, pb=pb, ci=ci: e.tensor_scalar(out=ca[:], in0=pb[:, 0:128], scalar1=cw[:, ci * 5:ci * 5 + 1], scalar2=None, op0=ALU.mult),
                       [pb, cw], [ca])
                    for j in range(1, 5):
                        D_(lambda e, ca=ca, pb=pb, ci=ci, j=j: e.scalar_tensor_tensor(out=ca[:], in0=pb[:, j:j + 128], scalar=cw[:, ci * 5 + j:ci * 5 + j + 1],
                                                                                   in1=ca[:], op0=ALU.mult, op1=ALU.add), [pb, cw, ca], [ca])
                    sg_ = B("csg%d" % ci, par)
                    A_(lambda e, sg_=sg_, ca=ca: e.activation(out=sg_[:], in_=ca[:], func=AF.Sigmoid), [ca], [sg_])
                    x = B("X%d" % ci, par)
                    G_(lambda e, x=x, ca=ca, sg_=sg_: e.tensor_tensor(out=x[:], in0=ca[:], in1=sg_[:], op=ALU.mult), [ca, sg_], [x])
                    X.append(x)
                for ci in range(2):
                    x = X[ci]
                    sq = B("sq%d" % ci, par)
                    A_(lambda e, sq=sq, x=x: e.activation(out=sq[:], in_=x[:], func=AF.Square), [x], [sq])
                    pn = pA[ci % 2]
                    P.mm(pn, pn[:, 0:128], ones_f, ones_f[:, :], sq, sq[:, :], True, True)
                    rn = B("rn%d" % ci, par)
                    A_(lambda e, rn=rn, pn=pn: e.activation(out=rn[:], in_=pn[:, 0:128], func=AF.Sqrt, bias=1e-6, scale=1.0), [pn], [rn])
                    D_(lambda e, rn=rn: e.reciprocal(out=rn[:], in_=rn[:]), [rn], [rn])
                    D_(lambda e, x=x, rn=rn: e.tensor_tensor(out=x[:], in0=x[:], in1=rn[:], op=ALU.mult), [x, rn], [x])
                qT, kT = X[0], X[1]
                if DBG.get('gcut', 9) < 1:
                    continue
                tok = []
                for ci in (1, 2, 3):
                    pt = pA[ci % 2]
                    P.tr(pt, pt[:, 0:128], X[ci], X[ci][:, :], cst, C(IDENT))
                    tb = B("tok%d" % ci, par)
                    A_(lambda e, tb=tb, pt=pt: e.activation(out=tb[:], in_=pt[:, 0:128], func=AF.Copy), [pt], [tb])
                    tok.append(tb)
                Kt, V = tok[0], tok[1:]
                if DBG.get('gcut', 9) < 2:
                    continue
                pab = pA[0]
                for k in range(KC):
                    P.mm(pab, pab[:, 0:8], u, u[:, k * GW + t0 - glo:k * GW + t0 - glo + 128], wabb, wabb[:, k * 8:(k + 1) * 8], k == 0, k == KC - 1)
                ab = B("ab", par, (128, 8))
                D_(lambda e, ab=ab, pab=pab: e.tensor_copy(out=ab[:], in_=pab[:, 0:8]), [pab], [ab])
                xa = B("xa", par, (128, 2)); ax = B("ax", par, (128, 2)); l1 = B("l1", par, (128, 2)); g2 = B("g2", par, (128, 2))
                be2 = B("be2", par, (128, 2))
                D_(lambda e, xa=xa, ab=ab, d=d: e.tensor_tensor(out=xa[:], in0=ab[:, d * 2:d * 2 + 2], in1=dtb[:, d * 2:d * 2 + 2], op=ALU.add), [ab, dtb], [xa])
                A_(lambda e, xa=xa, ax=ax: e.activation(out=ax[:], in_=xa[:], func=AF.Abs), [xa], [ax])
                A_(lambda e, ax=ax, l1=l1: e.activation(out=l1[:], in_=ax[:], func=AF.Exp, scale=-1.0), [ax], [l1])
                A_(lambda e, l1=l1: e.activation(out=l1[:], in_=l1[:], func=AF.Ln, bias=1.0, scale=1.0), [l1], [l1])
                D_(lambda e, xa=xa: e.tensor_scalar(out=xa[:], in0=xa[:], scalar1=0.0, scalar2=None, op0=ALU.max), [xa], [xa])
                D_(lambda e, xa=xa, l1=l1: e.tensor_tensor(out=xa[:], in0=xa[:], in1=l1[:], op=ALU.add), [xa, l1], [xa])
                D_(lambda e, xa=xa, g2=g2, d=d: e.tensor_tensor(out=g2[:], in0=xa[:], in1=negA[:, d * 2:d * 2 + 2], op=ALU.mult), [xa, negA], [g2])
                A_(lambda e, be2=be2, ab=ab, d=d: e.activation(out=be2[:], in_=ab[:, 4 + d * 2:4 + d * 2 + 2], func=AF.Sigmoid), [ab], [be2])
                pg = pA[1]
                P.mm(pg, pg[:, 0:2], cst, C(MC), g2, g2[:, :], True, True)
                P.mm(pg, pg[:, 2:4], cst, C(SAME), g2, g2[:, :], True, True)
                gsel = B("gsel", par, (128, 4))
                for cb in range(2):
                    D_(lambda e, gsel=gsel, g2=g2, cb=cb: e.tensor_scalar(out=gsel[:, cb * 2:cb * 2 + 2], in0=cind[:, :], scalar1=g2[:, cb:cb + 1], scalar2=None, op0=ALU.mult),
                       [cind, g2], [gsel])
                P.mm(pg, pg[:, 4:8], ones_f, ones_f[:, :], gsel, gsel[:, :], True, True)
                gcl = B("gcl", par, (128, 4))
                D_(lambda e, gcl=gcl, pg=pg: e.tensor_copy(out=gcl[:], in_=pg[:, 0:4]), [pg], [gcl])
                eglb = B("eglb", par, (128, 4))
                A_(lambda e, eglb=eglb, pg=pg: e.activation(out=eglb[:], in_=pg[:, 4:8], func=AF.Exp), [pg], [eglb])
                egc = B("egc", par, (128, 2)); bk2 = B("bk2", par, (128, 2)); kd2 = B("kd2", par, (128, 2)); qs2 = B("qs2", par, (128, 2))
                A_(lambda e, egc=egc, gcl=gcl: e.activation(out=egc[:], in_=gcl[:, 0:2], func=AF.Exp), [gcl], [egc])
                D_(lambda e, bk2=bk2, be2=be2, egc=egc: e.tensor_tensor(out=bk2[:], in0=be2[:], in1=egc[:], op=ALU.mult), [be2, egc], [bk2])
                D_(lambda e, kd2=kd2, gcl=gcl: e.tensor_tensor(out=kd2[:], in0=gcl[:, 2:4], in1=gcl[:, 0:2], op=ALU.subtract), [gcl], [kd2])
                A_(lambda e, kd2=kd2: e.activation(out=kd2[:], in_=kd2[:], func=AF.Exp), [kd2], [kd2])
                D_(lambda e, qs2=qs2, egc=egc: e.tensor_scalar(out=qs2[:], in0=egc[:], scalar1=RSQ, scalar2=None, op0=ALU.mult), [egc], [qs2])
                if DBG.get('gcut', 9) < 3:
                    continue
                pk = pM[0]
                P.mm(pk, pk[:, 0:128], kT, kT[:, :], kT, kT[:, :], True, True)
                KKm = B("KKm", par)
                D_(lambda e, KKm=KKm, pk=pk, MS=MS: e.tensor_tensor(out=KKm[:], in0=pk[:, 0:128], in1=C(MS), op=ALU.mult), [pk, cst], [KKm])
                pq = pM[1]
                P.mm(pq, pq[:, 0:128], kT, kT[:, :], qT, qT[:, :], True, True)
                QKTm = B("QKTm", par)
                D_(lambda e, QKTm=QKTm, pq=pq, MIT=MIT: e.scalar_tensor_tensor(out=QKTm[:], in0=pq[:, 0:128], scalar=RSQ, in1=C(MIT), op0=ALU.mult, op1=ALU.mult),
                   [pq, cst], [QKTm])
                if DBG.get('gcut', 9) < 4 or (DBG.get('gcut', 9) == 4 and DBG.get('sub', 0) == 0):
                    continue
                o_t = B("o_t", par, (128, 256))
                for cb in range(2):
                    sfx = "%d" % cb
                    gM = B("gM" + sfx, par)
                    D_(lambda e, gM=gM, g2=g2, cb=cb, MC=MC: e.tensor_scalar(out=gM[:], in0=C(MC), scalar1=g2[:, cb:cb + 1], scalar2=None, op0=ALU.mult), [cst, g2], [gM])
                    P.mm(pD, pD[:, 0:128], gM, gM[:, :], ones_f, ones_f[:, :], True, False)
                    P.mm(pD, pD[:, 0:128], negones, negones[:, :], gM, gM[:, :], False, True)
                    if DBG.get('gcut', 9) == 4 and DBG.get('sub', 0) == 1:
                        continue
                    E1 = B("E1" + sfx, par); E2 = B("E2" + sfx, par)
                    D_(lambda e, E1=E1: e.tensor_scalar(out=E1[:], in0=pD[:, 0:128], scalar1=0.0, scalar2=None, op0=ALU.min), [pD], [E1])
                    D_(lambda e, E2=E2: e.tensor_scalar(out=E2[:], in0=pD[:, 0:128], scalar1=0.0, scalar2=None, op0=ALU.max), [pD], [E2])
                    if DBG.get('gcut', 9) == 4 and DBG.get('sub', 0) == 2:
                        continue
                    A_(lambda e, E1=E1: e.activation(out=E1[:], in_=E1[:], func=AF.Exp), [E1], [E1])
                    A_(lambda e, E2=E2: e.activation(out=E2[:], in_=E2[:], func=AF.Exp, scale=-1.0), [E2], [E2])
                    if DBG.get('gcut', 9) < 5 and DBG.get('sub', 0) == 3:
                        continue
                    if DBG.get('gcut', 9) < 5:
                        continue
                    Al = [B("Aa" + sfx, par), B("Ab" + sfx, par)]
                    Bl = [B("Ba" + sfx, par), B("Bb" + sfx, par)]
                    Tl = [B("Ta" + sfx, par), B("Tb" + sfx, par)]
                    D_(lambda e, A0=Al[0], E1=E1, be2=be2, KKm=KKm, cb=cb: e.scalar_tensor_tensor(out=A0[:], in0=E1[:], scalar=be2[:, cb:cb + 1], in1=KKm[:],
                                                                                               op0=ALU.mult, op1=ALU.mult), [E1, be2, KKm], [Al[0]])
                    QKd = B("QKd" + sfx, par)
                    G_(lambda e, QKd=QKd, QKTm=QKTm, E2=E2: e.tensor_tensor(out=QKd[:], in0=QKTm[:], in1=E2[:], op=ALU.mult), [QKTm, E2], [QKd])
                    pt = pM[cb]
                    P.tr(pt, pt[:, 0:128], Al[0], Al[0][:, :], cst, C(IDENT))
                    A_(lambda e, B0=Bl[0], pt=pt: e.activation(out=B0[:], in_=pt[:, 0:128], func=AF.Copy), [pt], [Bl[0]])
                    D_(lambda e, T0=Tl[0], B0=Bl[0]: e.tensor_tensor(out=T0[:], in0=C(IDENT), in1=B0[:], op=ALU.subtract), [cst, Bl[0]], [Tl[0]])
                    for l in range(DBG.get('nl', 5)):
                        Ac, Bc, An, Bn = Al[l % 2], Bl[l % 2], Al[(l + 1) % 2], Bl[(l + 1) % 2]
                        Tc, Tn = Tl[l % 2], Tl[(l + 1) % 2]
                        p1 = pM[cb]
                        P.mm(p1, p1[:, 0:128], Bc, Bc[:, :], Ac, Ac[:, :], True, True)
                        if l < 4:
                            P.mm(p1, p1[:, 128:256], Ac, Ac[:, :], Bc, Bc[:, :], True, True)
                        A_(lambda e, An=An, p1=p1: e.activation(out=An[:], in_=p1[:, 0:128], func=AF.Copy), [p1], [An])
                        if l < 4:
                            A_(lambda e, Bn=Bn, p1=p1: e.activation(out=Bn[:], in_=p1[:, 128:256], func=AF.Copy), [p1], [Bn])
                        P.mm(p1, p1[:, 256:384], An, An[:, :], Tc, Tc[:, :], True, True)
                        D_(lambda e, Tn=Tn, Tc=Tc, p1=p1: e.tensor_tensor(out=Tn[:], in0=p1[:, 256:384], in1=Tc[:], op=ALU.add), [Tc, p1], [Tn])
                    Tt = Tl[1]
                    if DBG.get('gcut', 9) < 6:
                        continue
                    RHv = B("RHv" + sfx, par); RHk = B("RHk" + sfx, par); kdec = B("kdec" + sfx, par)
                    D_(lambda e, RHv=RHv, Vh=V[cb], be2=be2, cb=cb: e.tensor_scalar(out=RHv[:], in0=Vh[:], scalar1=be2[:, cb:cb + 1], scalar2=None, op0=ALU.mult), [V[cb], be2], [RHv])
                    D_(lambda e, RHk=RHk, Kt=Kt, bk2=bk2, cb=cb: e.tensor_scalar(out=RHk[:], in0=Kt[:], scalar1=bk2[:, cb:cb + 1], scalar2=None, op0=ALU.mult), [Kt, bk2], [RHk])
                    D_(lambda e, kdec=kdec, Kt=Kt, kd2=kd2, cb=cb: e.tensor_scalar(out=kdec[:], in0=Kt[:], scalar1=kd2[:, cb:cb + 1], scalar2=None, op0=ALU.mult), [Kt, kd2], [kdec])
                    P.mm(pU, pU[:, 0:128], Tt, Tt[:, :], RHv, RHv[:, :], True, True)
                    P.mm(pU, pU[:, 128:256], RHk, RHk[:, :], Tt, Tt[:, :], True, True)
                    un = B("un" + sfx, par); wdT = B("wdT" + sfx, par)
                    A_(lambda e, un=un: e.activation(out=un[:], in_=pU[:, 0:128], func=AF.Copy), [pU], [un])
                    A_(lambda e, wdT=wdT: e.activation(out=wdT[:], in_=pU[:, 128:256], func=AF.Copy), [pU], [wdT])
                    if DBG.get('gcut', 9) < 7:
                        continue
                    vn = B("vn" + sfx, par); t1 = B("t1" + sfx, par)
                    Sb = S[(d, cb)]
                    for ch in ((0, 1) if d == 0 else (1, 0)):
                        r = slice(ch * 64, (ch + 1) * 64)
                        P.mm(pR, pR[r, 0:128], wdT, wdT[:, r], Sb, Sb[:, :], True, True)
                        D_(lambda e, vn=vn, un=un, r=r: e.scalar_tensor_tensor(out=vn[r, :], in0=pR[r, 0:128], scalar=-1.0, in1=un[r, :], op0=ALU.mult, op1=ALU.add), [un, pR], [vn])
                        P.mm(pR, pR[r, 128:256], qT, qT[:, r], Sb, Sb[:, :], True, True)
                        P.mm(pR, pR[r, 256:384], QKd, QKd[r, r], vn, vn[r, :], True, True)
                        A_(lambda e, t1=t1, r=r, qs2=qs2, cb=cb: e.activation(out=t1[r, :], in_=pR[r, 128:256], func=AF.Copy, scale=qs2[r, cb:cb + 1]), [pR, qs2], [t1])
                        D_(lambda e, o_t=o_t, t1=t1, r=r, cb=cb: e.tensor_tensor(out=o_t[r, cb * 128:(cb + 1) * 128], in0=pR[r, 256:384], in1=t1[r, :], op=ALU.add), [t1, pR], [o_t])
                        P.mm(pKVb, pKVb[:, kvo:kvo + 128], kdec, kdec[r, :], vn, vn[r, :], True, True)
                        col = cb * 2 + ch
                        D_(lambda e, Sb=Sb, eglb=eglb, col=col: e.tensor_scalar(out=Sb[:], in0=Sb[:], scalar1=eglb[:, col:col + 1], scalar2=None, op0=ALU.mult), [Sb, eglb], [Sb])
                        D_(lambda e, Sb=Sb: e.tensor_tensor(out=Sb[:], in0=pKVb[:, kvo:kvo + 128], in1=Sb[:], op=ALU.add), [Sb, pKVb], [Sb])
                if DBG.get('gcut', 9) < 8:
                    continue
                if dbg_d is not None and d == DBG.get("gdbg_d", 0):
                    for (bb, c0, wdt) in ((X[0], 0, 128), (X[1], 128, 128), (X[2], 256, 128), (X[3], 384, 128), (ab, 512, 8), (g2, 520, 2), (be2, 522, 2),
                                          (gcl, 524, 4), (eglb, 528, 4), (o_t, 532, 256), (bufs[("Tb0", par)], 788, 128), (bufs[("un0", par)], 916, 108)):
                        P.dma('sp', dbg_d, dbg_d[t0:t0 + 128, c0:c0 + wdt], bb, bb[:, 0:wdt], disjoint=True)
                if d == 0:
                    P.dma('sp', of_s, of_s[t0:t0 + 128, :], o_t, o_t[:], disjoint=True)
                else:
                    of_t = B("of_t", par, (128, 256))
                    P.dma('sp', of_t, of_t[:], of_s, of_s[t0:t0 + 128, :])
                    pz = pA[0]
                    for k in range(KC):
                        P.mm(pz, pz[:, 0:256], u, u[:, k * GW + t0 - glo:k * GW + t0 - glo + 128], wzb, wzb[:, k * 256:(k + 1) * 256], k == 0, k == KC - 1)
                    sgz = B("sgz", par, (128, 256)); zs = B("zs", par, (128, 256))
                    A_(lambda e, sgz=sgz, pz=pz: e.activation(out=sgz[:], in_=pz[:, 0:256], func=AF.Sigmoid), [pz], [sgz])
                    D_(lambda e, zs=zs, sgz=sgz, pz=pz: e.tensor_tensor(out=zs[:], in0=pz[:, 0:256], in1=sgz[:], op=ALU.mult), [pz, sgz], [zs])
                    D_(lambda e, o_t=o_t, of_t=of_t: e.tensor_tensor(out=o_t[:], in0=o_t[:], in1=of_t[:], op=ALU.add), [o_t, of_t], [o_t])
                    ss = B("ss", par, (128, 2)); junk = B("junk", par, (128, 128)); y = B("y", par, (128, 256))
                    for hl in range(2):
                        A_(lambda e, junk=junk, o_t=o_t, ss=ss, hl=hl: e.activation(out=junk[:], in_=o_t[:, hl * 128:(hl + 1) * 128], func=AF.Square, accum_out=ss[:, hl:hl + 1]),
                           [o_t], [junk, ss])
                    A_(lambda e, ss=ss: e.activation(out=ss[:], in_=ss[:], func=AF.Sqrt, bias=EPS, scale=1.0 / 128), [ss], [ss])
                    D_(lambda e, ss=ss: e.reciprocal(out=ss[:], in_=ss[:]), [ss], [ss])
                    for hl in range(2):
                        D_(lambda e, y=y, o_t=o_t, ss=ss, hl=hl: e.scalar_tensor_tensor(out=y[:, hl * 128:(hl + 1) * 128], in0=o_t[:, hl * 128:(hl + 1) * 128],
                                                                                      scalar=ss[:, hl:hl + 1], in1=nwb[:, :], op0=ALU.mult, op1=ALU.mult), [o_t, ss, nwb], [y])
                    D_(lambda e, y=y, zs=zs: e.tensor_tensor(out=y[:], in0=y[:], in1=zs[:], op=ALU.mult), [y, zs], [y])
                    yT = B("yT", par, (128, 256), BF16)
                    for hl in range(2):
                        pt = pA[1]
                        P.tr(pt, pt[:, 0:128], y, y[:, hl * 128:(hl + 1) * 128], cst, C(IDENT))
                        A_(lambda e, yT=yT, pt=pt, hl=hl: e.activation(out=yT[:, hl * 128:(hl + 1) * 128], in_=pt[:, 0:128], func=AF.Copy), [pt], [yT])
                        P.dma('sp', oT_d, oT_d[hl * 128:(hl + 1) * 128, t0:t0 + 128], yT, yT[:, hl * 128:(hl + 1) * 128], disjoint=True)
    cst_np, cind_np = gdn_consts()
    in_maps = []
    for c in range(NCORES):
        hv = [2 * c, 2 * c + 1]
        chunks = [w_in[:, c * 128:(c + 1) * 128], w_in[:, 1024 + c * 128:1024 + (c + 1) * 128],
                  w_in[:, 2048 + hv[0] * 128:2048 + (hv[0] + 1) * 128], w_in[:, 2048 + hv[1] * 128:2048 + (hv[1] + 1) * 128]]
        wnp = np.stack([x.reshape(KC, 128, 128) for x in chunks], 0).reshape(4 * KC, 128, 128)
        wz = w_in[:, 4096 + hv[0] * 128:4096 + (hv[1] + 1) * 128].reshape(KC, 128, 256)
        abcols = [6144 + kind * 32 + dd * 16 + h for kind in range(2) for dd in range(2) for h in hv]
        wab = w_in[:, abcols].reshape(KC, 128, 8)
        ch_idx = [np.arange(c * 128, (c + 1) * 128), 1024 + np.arange(c * 128, (c + 1) * 128),
                  2048 + np.arange(hv[0] * 128, (hv[0] + 1) * 128), 2048 + np.arange(hv[1] * 128, (hv[1] + 1) * 128)]
        cwn = np.concatenate([conv_w[:, idx].T for idx in ch_idx], 1)
        al = np.array([a_log[dd, h] for dd in range(2) for h in hv], np.float32)
        dtv = np.array([dt_bias[dd, h] for dd in range(2) for h in hv], np.float32)
        in_maps.append({"uT": np.ascontiguousarray(uT_all), "w": np.ascontiguousarray(wnp, dtype=np.float32),
                        "wz": np.ascontiguousarray(wz, dtype=np.float32), "wab": np.ascontiguousarray(wab, dtype=np.float32),
                        "cw": np.ascontiguousarray(cwn, dtype=np.float32), "cst": cst_np, "cind": cind_np,
                        "alog": _bc(al), "dtb": _bc(dtv), "nw": _bc(norm_w)})
    res = P.run(in_maps)
    if DBG.get("gdbg"):
        DBG["dbg_out"] = [res[c]["dbg"] for c in range(NCORES)]
    return np.concatenate([res[c]["oT"] for c in range(NCORES)], 0)


def _tok_index(c, LC):
    return np.concatenate([np.arange(CTX), CTX + c * LC + np.arange(LC)])


def _gather_T(per_core, LC, NT):
    cols = []
    for c in range(NCORES):
        a = np.asarray(per_core[c]).reshape(NT, 128, KC, 128).transpose(2, 1, 0, 3).reshape(D, NT * 128)
        cols.append(a[:, CTX:] if c > 0 else a)
    return np.ascontiguousarray(np.concatenate(cols, 1))


def kernel(x, c, ctx, c_ctx, ada_w, ada_b, ln_w, ln_b, gdn_w_in, gdn_conv_w, gdn_a_log, gdn_dt_bias,
           gdn_norm_w, gdn_w_out, diff_w_in, diff_lambda, diff_subln_w, diff_w_out, router_w, router_b,
           moe_w_gate_up, moe_b_gate_up, moe_w_down, moe_b_down):
    f32 = lambda a: np.asarray(a, dtype=np.float32)
    x, c, ctx, c_ctx = f32(x), f32(c), f32(ctx), f32(c_ctx)
    L = x.shape[1]
    LC = L // NCORES
    NT = (CTX + LC) // 128
    TT = CTX + L
    mods = launch_mods(c[0], c_ctx, f32(ada_w), f32(ada_b))

    def m(l, r, i):
        return mods[l, r, i * D:(i + 1) * D]
    h_cores = [np.concatenate([ctx[0], x[0, cc * LC:(cc + 1) * LC]], 0) for cc in range(NCORES)]
    partials = None
    prev = None
    for l in range(DEPTH):
        j = l // 2
        cur = dict(m0_lat=m(l, 0, 0), m1_lat=m(l, 0, 1), m0_ctx=m(l, 1, 0), m1_ctx=m(l, 1, 1))
        h_cores, uT = launch_tokA(h_cores, partials, prev, cur, NT)
        uT_all = _gather_T(uT, LC, NT)
        if l % 2 == 0:
            oT_all = launch_gdn(uT_all, f32(gdn_w_in[j]), f32(gdn_conv_w[j]), f32(gdn_a_log[j]), f32(gdn_dt_bias[j]), f32(gdn_norm_w[j]), TT)
            HK = 16
            wout = f32(gdn_w_out[j])
        else:
            lambda_init = 0.8 - 0.6 * math.exp(-0.3 * l)
            oT_all = launch_attn(uT_all, f32(diff_w_in[j]), f32(diff_lambda[j]), f32(diff_subln_w[j]), lambda_init, TT)
            HK = 8
            wout = f32(diff_w_out[j])
        oT_cores = []
        for cc in range(NCORES):
            a = oT_all[:, _tok_index(cc, LC)]
            oT_cores.append(np.ascontiguousarray(a.reshape(HK, 128, NT, 128).transpose(2, 1, 0, 3).reshape(NT, 128, HK * 128)))
        mod = dict(m2_lat=m(l, 0, 2), m2_ctx=m(l, 1, 2), lnw=f32(ln_w[l, 0]), lnb=f32(ln_b[l, 0]),
                   m3_lat=m(l, 0, 3), m4_lat=m(l, 0, 4), m3_ctx=m(l, 1, 3), m4_ctx=m(l, 1, 4))
        h_cores, vT, gates = launch_tokB(h_cores, oT_cores, wout, mod, f32(router_w[l]), f32(router_b[l]), NT, HK)
        vT_all = _gather_T(vT, LC, NT)
        gates_all = np.concatenate([gates[0]] + [gates[cc][CTX:] for cc in range(1, NCORES)], 0)
        parts = launch_moe(vT_all, gates_all, f32(moe_w_gate_up[l]), f32(moe_b_gate_up[l]), f32(moe_w_down[l]), f32(moe_b_down[l]), TT)
        partials = [np.stack([parts[r][_tok_index(cc, LC)] for r in range(NCORES)], 0) for cc in range(NCORES)]
        prev = dict(m5_lat=m(l, 0, 5), m5_ctx=m(l, 1, 5), lnw=f32(ln_w[l, 1]), lnb=f32(ln_b[l, 1]))
    h_cores, _ = launch_tokA(h_cores, partials, prev, None, NT)
    out = np.concatenate([h_cores[cc][CTX:] for cc in range(NCORES)], 0)[None]
    return np.ascontiguousarray(out, dtype=np.float32)
```

```python
import math
from contextlib import ExitStack
import numpy as np
import ml_dtypes
import concourse.bass as bass
import concourse.mybir as mybir
from concourse.bass_utils import run_bass_kernel_spmd

F32 = mybir.dt.float32
BF16 = mybir.dt.bfloat16
AF = mybir.ActivationFunctionType
ALU = mybir.AluOpType
AX = mybir.AxisListType
NPBF = ml_dtypes.bfloat16

NCORES = 8
D = 1024
KC = D // 128
CTX = 256
DEPTH = 4
NEXP = 32
EPC = NEXP // NCORES
ALPHA = (2 * DEPTH) ** 0.25
EPS = 1e-5

LAUNCHES = [0]
PENG = 'pool'
DBG = {}


class Buf:
    def __init__(self, t, name, space):
        self.t = t
        self.name = name
        self.space = space
        self.lw = None
        self.rd = []
        self.dsem = None
        self.dcnt = 0

    def __getitem__(self, idx):
        return self.t[idx]


class Prog:
    ENG = ['pe', 'act', 'dve', 'pool', 'sp']

    def __init__(self):
        self.nc = bass.Bass("TRN2", target_bir_lowering=False)
        self.st = ExitStack()
        self.q = {e: [] for e in self.ENG}
        self.cnt = {e: 0 for e in self.ENG}
        self.seen = {e: {} for e in self.ENG}
        self.esem = {e: self.st.enter_context(self.nc.semaphore("es_" + e)) for e in self.ENG}
        self.outs = []
        self.nid = 0

    def sb(self, name, shape, dt=F32):
        t = self.st.enter_context(self.nc.sbuf_tensor(name, list(shape), dt))
        return Buf(t, name, 'sb')

    def ps(self, name, shape, dt=F32):
        t = self.st.enter_context(self.nc.psum_tensor(name, list(shape), dt))
        return Buf(t, name, 'ps')

    def din(self, name, shape, dt=F32):
        return Buf(self.nc.dram_tensor(name, list(shape), dt, kind="ExternalInput").ap(), name, 'dram')

    def dout(self, name, shape, dt=F32):
        b = Buf(self.nc.dram_tensor(name, list(shape), dt, kind="ExternalOutput").ap(), name, 'dram')
        self.outs.append(b)
        return b

    def dscratch(self, name, shape, dt=F32):
        return Buf(self.nc.dram_tensor(name, list(shape), dt).ap(), name, 'dram')

    def _need(self, eng, tok, waits, raw=False):
        if tok is None:
            return
        if tok[0] == 'e':
            if tok[1] == eng and (eng == 'pe' or not raw):
                return
            key = ('e', tok[1])
            sem = self.esem[tok[1]]
        else:
            key = ('d', id(tok[1]))
            sem = tok[1].dsem
        val = tok[2]
        if self.seen[eng].get(key, 0) >= val:
            return
        self.seen[eng][key] = val
        waits.append((sem, val))

    def op(self, eng, fn, reads=(), writes=()):
        waits = []
        for b in reads:
            self._need(eng, b.lw, waits, raw=True)
            if b.space == 'ps':
                for r in b.rd:
                    self._need(eng, r, waits)
        for b in writes:
            self._need(eng, b.lw, waits)
            for r in b.rd:
                self._need(eng, r, waits)
        self.cnt[eng] += 1
        tok = ('e', eng, self.cnt[eng])
        for b in reads:
            b.rd.append(tok)
            if len(b.rd) > 64:
                b.rd = b.rd[-64:] if False else b.rd
        for b in writes:
            b.lw = tok
            b.rd = []
        self.q[eng].append((waits, fn, None))

    def dma(self, qeng, dst, dst_ap, src, src_ap, disjoint=False):
        waits = []
        self._need(qeng, src.lw, waits)
        if not disjoint:
            self._need(qeng, dst.lw, waits)
            for r in dst.rd:
                self._need(qeng, r, waits)
        if dst.dsem is None:
            dst.dsem = self.st.enter_context(self.nc.semaphore("ds_%s_%d" % (dst.name, self.nid)))
            self.nid += 1
        dst.dcnt += 16
        tok = ('d', dst, dst.dcnt)
        src.rd.append(tok)
        dst.lw = tok
        dst.rd = []
        self.q[qeng].append((waits, lambda e: e.dma_start(out=dst_ap, in_=src_ap), dst))

    def build(self):
        nc = self.nc
        fin = []
        for b in self.outs:
            if b.dsem is not None:
                fin.append((b.dsem, b.dcnt))
        with nc.Block() as block:
            def mk(ename):
                def body(engine):
                    for waits, fn, dbuf in self.q[ename]:
                        for sem, val in waits:
                            engine.wait_ge(sem, val)
                        ins = fn(engine)
                        if dbuf is None:
                            ins.then_inc(self.esem[ename], 1)
                        else:
                            ins.then_inc(dbuf.dsem, 16)
                    if ename == 'sp':
                        for sem, val in fin:
                            engine.wait_ge(sem, val)
                return body
            block.tensor(mk('pe'))
            block.scalar(mk('act'))
            block.vector(mk('dve'))
            block.gpsimd(mk('pool'))
            block.sync(mk('sp'))
        self.st.close()
        return nc

    def mm(self, out, out_ap, lhsT, lhsT_ap, rhs, rhs_ap, start, stop):
        rd = [lhsT, rhs] + ([] if start else [out])
        self.op('pe', lambda e: e.matmul(out_ap, lhsT=lhsT_ap, rhs=rhs_ap, start=start, stop=stop),
                reads=rd, writes=[out])

    def tr(self, out, out_ap, in_, in_ap, ident, ident_ap):
        self.op('pe', lambda e: e.transpose(out_ap, in_ap, ident_ap), reads=[in_, ident], writes=[out])

    def run(self, in_maps):
        import time as _t
        t0 = _t.time()
        nc = self.build()
        t1 = _t.time()
        LAUNCHES[0] += 1
        res = run_bass_kernel_spmd(nc, in_maps, core_ids=list(range(NCORES)), **({'trace': True} if DBG.get('trace') else {}))
        t2 = _t.time()
        if DBG.get('trace'):
            print('[exec_time_ns]', res.exec_time_ns, flush=True)
        if DBG.get('timing', 1):
            nb = sum(v.nbytes for m_ in in_maps for v in m_.values())
            ob = sum(v.nbytes for r_ in res.results for v in r_.values())
            print("[launch %d] build %.1fs run %.1fs in %.0fMB out %.0fMB ninstr %d" % (
                LAUNCHES[0], t1 - t0, t2 - t1, nb / 1e6, ob / 1e6, sum(len(q) for q in self.q.values())), flush=True)
        return res.results


def consts_np():
    c = {}
    c['ident_f'] = np.eye(128, dtype=np.float32)
    c['ident_b'] = np.eye(128, dtype=np.float32).astype(NPBF)
    return c


def launch_mods(c, c_ctx, ada_w, ada_b):
    NW = 6 * D // NCORES
    P = Prog()
    cs_d = P.din("cs", [128, KC * 2])
    w_d = P.din("w", [DEPTH * KC, 128, NW])
    b_d = P.din("b", [1, DEPTH * NW])
    out_d = P.dout("mods", [2, DEPTH * NW])
    cs = P.sb("cs_s", [128, KC * 2])
    sg = P.sb("sg_s", [128, KC * 2])
    s = P.sb("s_s", [128, KC * 2])
    bs = P.sb("b_s", [1, DEPTH * NW])
    ones = P.sb("ones", [1, 2])
    osb = P.sb("o_s", [2, DEPTH * NW])
    wt = [P.sb("w%d" % i, [128, NW]) for i in range(4)]
    pp = [P.ps("pp%d" % i, [2, 512]) for i in range(4)]
    P.dma('sp', cs, cs[:], cs_d, cs_d[:, :])
    P.dma('sp', bs, bs[:], b_d, b_d[:, :])
    P.op('dve', lambda e: e.memset(ones[:], 1.0), writes=[ones])
    P.op('act', lambda e: e.activation(out=sg[:], in_=cs[:], func=AF.Sigmoid), reads=[cs], writes=[sg])
    P.op('dve', lambda e: e.tensor_tensor(out=s[:], in0=cs[:], in1=sg[:], op=ALU.mult), reads=[cs, sg], writes=[s])
    n = 0
    for l in range(DEPTH):
        for k in range(KC):
            w = wt[n % 4]
            P.dma('sp', w, w[:], w_d, w_d[l * KC + k, :, :])
            for h in range(2):
                pt = pp[(l % 2) * 2 + h]
                P.mm(pt, pt[:, 0:384], s, s[:, 2 * k:2 * k + 2], w, w[:, h * 384:(h + 1) * 384], k == 0, False)
            n += 1
        for h in range(2):
            pt = pp[(l % 2) * 2 + h]
            c0 = l * NW + h * 384
            P.mm(pt, pt[:, 0:384], ones, ones[:, :], bs, bs[:, c0:c0 + 384], False, True)
            P.op('dve', lambda e, pt=pt, c0=c0: e.tensor_copy(out=osb[:, c0:c0 + 384], in_=pt[:, 0:384]),
                 reads=[pt], writes=[osb])
    P.dma('sp', out_d, out_d[:, :], osb, osb[:])
    cs_np = np.ascontiguousarray(
        np.stack([c.reshape(KC, 128), c_ctx.reshape(KC, 128)], -1).transpose(1, 0, 2).reshape(128, KC * 2))
    in_maps = []
    for r in range(NCORES):
        wsh = ada_w[:, :, r * NW:(r + 1) * NW].reshape(DEPTH * KC, 128, NW)
        bsh = ada_b[:, r * NW:(r + 1) * NW].reshape(1, DEPTH * NW)
        in_maps.append({"cs": cs_np, "w": np.ascontiguousarray(wsh), "b": np.ascontiguousarray(bsh)})
    res = P.run(in_maps)
    mods = np.zeros((DEPTH, 2, 6 * D), np.float32)
    for r in range(NCORES):
        o = res[r]["mods"].reshape(2, DEPTH, NW)
        for l in range(DEPTH):
            mods[l, :, r * NW:(r + 1) * NW] = o[:, l, :]
    return mods


def _ln_mod(P, W, src, lnw, lnb, sc1, sh, out_h, out_u):
    ag, rstd, tmp, junk = W['ag'], W['rstd'], W['tmp'], W['junk']
    P.op('act', lambda e: e.activation(out=junk[:], in_=src[:], func=AF.Copy, accum_out=ag[:, 0:1]), reads=[src], writes=[junk, ag])
    P.op('act', lambda e: e.activation(out=junk[:], in_=src[:], func=AF.Square, accum_out=ag[:, 1:2]), reads=[src], writes=[junk, ag])
    P.op('dve', lambda e: e.tensor_scalar(out=ag[:], in0=ag[:], scalar1=1.0 / D, scalar2=None, op0=ALU.mult), reads=[ag], writes=[ag])
    P.op('dve', lambda e: e.tensor_tensor(out=rstd[:], in0=ag[:, 0:1], in1=ag[:, 0:1], op=ALU.mult), reads=[ag], writes=[rstd])
    P.op('dve', lambda e: e.scalar_tensor_tensor(out=rstd[:], in0=ag[:, 1:2], scalar=EPS, in1=rstd[:], op0=ALU.add, op1=ALU.subtract),
         reads=[ag, rstd], writes=[rstd])
    P.op('act', lambda e: e.activation(out=rstd[:], in_=rstd[:], func=AF.Sqrt), reads=[rstd], writes=[rstd])
    P.op('dve', lambda e: e.reciprocal(out=rstd[:], in_=rstd[:]), reads=[rstd], writes=[rstd])
    P.op('dve', lambda e: e.tensor_scalar(out=tmp[:], in0=src[:], scalar1=ag[:, 0:1], scalar2=rstd[:, 0:1],
                                          op0=ALU.subtract, op1=ALU.mult), reads=[src, ag, rstd], writes=[tmp])
    P.op(PENG, lambda e: e.tensor_tensor(out=tmp[:], in0=tmp[:], in1=lnw[:], op=ALU.mult), reads=[tmp, lnw], writes=[tmp])
    P.op(PENG, lambda e: e.tensor_tensor(out=out_h[:], in0=tmp[:], in1=lnb[:], op=ALU.add), reads=[tmp, lnb], writes=[out_h])
    if out_u is not None:
        P.op('dve', lambda e: e.tensor_tensor(out=tmp[:], in0=out_h[:], in1=sc1[:], op=ALU.mult), reads=[out_h, sc1], writes=[tmp])
        P.op('dve', lambda e: e.tensor_tensor(out=out_u[:], in0=tmp[:], in1=sh[:], op=ALU.add), reads=[tmp, sh], writes=[out_u])


def _bc(v):
    return np.ascontiguousarray(np.broadcast_to(v.reshape(1, -1), (128, v.size))).astype(np.float32)


def launch_tokA(h_cores, partials, prev, cur, NT):
    P = Prog()
    R = NT * 128
    h_d = P.din("h", [R, D])
    ident_d = P.din("ident", [128, 128])
    ident = P.sb("ident_s", [128, 128])
    P.dma('sp', ident, ident[:], ident_d, ident_d[:, :])
    names = []
    if prev is not None:
        names += ['m5_lat', 'm5_ctx', 'lnw', 'lnb']
        p_d = P.din("part", [8, R, D], BF16)
    if cur is not None:
        names += ['m0_lat', 'm1_lat', 'm0_ctx', 'm1_ctx']
        uT_d = P.dout("uT", [NT, 128, D], BF16)
    if prev is not None:
        hn_d = P.dout("hn", [R, D])
    mod_d = P.din("modb", [128, max(1, len(names)) * D])
    mb = {}
    for i, nm in enumerate(names):
        mb[nm] = P.sb("mb_" + nm, [128, D])
        P.dma('sp', mb[nm], mb[nm][:], mod_d, mod_d[:, i * D:(i + 1) * D])
    if cur is not None:
        for nm in ('m1_lat', 'm1_ctx'):
            P.op('dve', lambda e, nm=nm: e.tensor_scalar(out=mb[nm][:], in0=mb[nm][:], scalar1=1.0, scalar2=None, op0=ALU.add),
                 reads=[mb[nm]], writes=[mb[nm]])
    W = dict(junk=P.sb("junk", [128, D]), ag=P.sb("ag", [128, 2]), rstd=P.sb("rstd", [128, 1]), tmp=P.sb("tmp", [128, D]))
    hb = [P.sb("hb%d" % i, [128, D]) for i in range(2)]
    pb = [P.sb("pb%d" % i, [128, 8 * D], BF16) for i in range(2)] if prev is not None else None
    acc = P.sb("acc", [128, D])
    acc2 = P.sb("acc2", [128, D])
    hn = [P.sb("hn%d" % i, [128, D]) for i in range(2)]
    ub = [P.sb("ub%d" % i, [128, D]) for i in range(2)]
    utb = [P.sb("utb%d" % i, [128, D], BF16) for i in range(2)]
    ptr = [P.ps("ptr%d" % i, [128, D]) for i in range(2)]
    for t in range(NT):
        sfx = 'ctx' if t < CTX // 128 else 'lat'
        h = hb[t % 2]
        P.dma('sp', h, h[:], h_d, h_d[t * 128:(t + 1) * 128, :])
        if prev is not None:
            pt = pb[t % 2]
            P.dma('sp', pt, pt[:].rearrange("p (r d) -> p r d", r=8), p_d,
                  p_d[:, t * 128:(t + 1) * 128, :].rearrange("r p d -> p r d"))
            P.op('dve', lambda e, pt=pt: e.tensor_tensor(out=acc[:], in0=pt[:, 0:D], in1=pt[:, D:2 * D], op=ALU.add), reads=[pt], writes=[acc])
            P.op(PENG, lambda e, pt=pt: e.tensor_tensor(out=acc2[:], in0=pt[:, 4 * D:5 * D], in1=pt[:, 5 * D:6 * D], op=ALU.add), reads=[pt], writes=[acc2])
            for r in (2, 3):
                P.op('dve', lambda e, pt=pt, r=r: e.tensor_tensor(out=acc[:], in0=acc[:], in1=pt[:, r * D:(r + 1) * D], op=ALU.add), reads=[pt, acc], writes=[acc])
            for r in (6, 7):
                P.op(PENG, lambda e, pt=pt, r=r: e.tensor_tensor(out=acc2[:], in0=acc2[:], in1=pt[:, r * D:(r + 1) * D], op=ALU.add), reads=[pt, acc2], writes=[acc2])
            P.op('dve', lambda e: e.tensor_tensor(out=acc[:], in0=acc[:], in1=acc2[:], op=ALU.add), reads=[acc, acc2], writes=[acc])
            m5 = mb['m5_' + sfx]
            P.op('dve', lambda e, m5=m5: e.tensor_tensor(out=acc[:], in0=acc[:], in1=m5[:], op=ALU.mult), reads=[acc, m5], writes=[acc])
            P.op('dve', lambda e, h=h: e.scalar_tensor_tensor(out=acc[:], in0=h[:], scalar=ALPHA, in1=acc[:], op0=ALU.mult, op1=ALU.add),
                 reads=[h, acc], writes=[acc])
            hnew = hn[t % 2]
            u = ub[t % 2] if cur is not None else None
            _ln_mod(P, W, acc, mb['lnw'], mb['lnb'], mb.get('m1_' + sfx), mb.get('m0_' + sfx), hnew, u)
            P.dma('sp', hn_d, hn_d[t * 128:(t + 1) * 128, :], hnew, hnew[:], disjoint=True)
        else:
            u = ub[t % 2]
            tmp = W['tmp']
            P.op('dve', lambda e, h=h, sfx=sfx: e.tensor_tensor(out=tmp[:], in0=h[:], in1=mb['m1_' + sfx][:], op=ALU.mult), reads=[h, mb['m1_' + sfx]], writes=[tmp])
            P.op('dve', lambda e, u=u, sfx=sfx: e.tensor_tensor(out=u[:], in0=tmp[:], in1=mb['m0_' + sfx][:], op=ALU.add), reads=[tmp, mb['m0_' + sfx]], writes=[u])
        if cur is not None:
            pz = ptr[t % 2]
            for k in range(KC):
                P.tr(pz, pz[:, k * 128:(k + 1) * 128], u, u[:, k * 128:(k + 1) * 128], ident, ident[:])
            ut = utb[t % 2]
            P.op('act', lambda e, ut=ut, pz=pz: e.activation(out=ut[:], in_=pz[:], func=AF.Copy), reads=[pz], writes=[ut])
            P.dma('sp', uT_d, uT_d[t, :, :], ut, ut[:], disjoint=True)
    in_maps = []
    for c in range(NCORES):
        m = {"h": np.ascontiguousarray(h_cores[c], dtype=np.float32), "ident": np.eye(128, dtype=np.float32)}
        src = {}
        if prev is not None:
            src.update(prev)
            m["part"] = np.ascontiguousarray(partials[c])
        if cur is not None:
            src.update(cur)
        m["modb"] = np.concatenate([_bc(src[nm]) for nm in names], 1) if names else np.zeros((128, D), np.float32)
        in_maps.append(m)
    res = P.run(in_maps)
    h_new = [res[c]["hn"] for c in range(NCORES)] if prev is not None else h_cores
    uT = [res[c]["uT"] for c in range(NCORES)] if cur is not None else None
    return h_new, uT


def launch_tokB(h_cores, oT_cores, wout, mod, rw, rb, NT, HK):
    P = Prog()
    R = NT * 128
    names = ['m2_lat', 'm2_ctx', 'lnw', 'lnb', 'm3_lat', 'm4_lat', 'm3_ctx', 'm4_ctx']
    h_d = P.din("h", [R, D])
    oT_d = P.din("oT", [NT, 128, HK * 128], BF16)
    w_d = P.din("wout", [HK, 128, D])
    mod_d = P.din("modb", [128, len(names) * D])
    rw_d = P.din("rw", [128, KC * NEXP])
    rb_d = P.din("rb", [1, NEXP])
    ident_d = P.din("ident", [128, 128])
    h1_d = P.dout("h1", [R, D])
    vT_d = P.dout("vT", [NT, 128, D], BF16)
    g_d = P.dout("gates", [R, NEXP])
    ident = P.sb("ident_s", [128, 128])
    P.dma('sp', ident, ident[:], ident_d, ident_d[:, :])
    rws = P.sb("rw_s", [128, KC * NEXP])
    P.dma('sp', rws, rws[:], rw_d, rw_d[:, :])
    rbs = P.sb("rb_s", [1, NEXP])
    P.dma('sp', rbs, rbs[:], rb_d, rb_d[:, :])
    ones = P.sb("ones", [1, 128])
    P.op('dve', lambda e: e.memset(ones[:], 1.0), writes=[ones])
    mb = {}
    for i, nm in enumerate(names):
        mb[nm] = P.sb("mb_" + nm, [128, D])
        P.dma('sp', mb[nm], mb[nm][:], mod_d, mod_d[:, i * D:(i + 1) * D])
    for nm in ('m4_lat', 'm4_ctx'):
        P.op('dve', lambda e, nm=nm: e.tensor_scalar(out=mb[nm][:], in0=mb[nm][:], scalar1=1.0, scalar2=None, op0=ALU.add),
             reads=[mb[nm]], writes=[mb[nm]])
    wob = P.sb("wob", [128, HK * D], BF16)
    wst = [P.sb("wst%d" % i, [128, D]) for i in range(2)]
    for k in range(HK):
        ws = wst[k % 2]
        P.dma('sp', ws, ws[:], w_d, w_d[k, :, :])
        eng = 'act' if (k % 2 == 0 or DBG.get('nopoolcast')) else 'pool'
        if eng == 'act':
            P.op('act', lambda e, ws=ws, k=k: e.activation(out=wob[:, k * D:(k + 1) * D], in_=ws[:], func=AF.Copy), reads=[ws], writes=[wob])
        else:
            P.op('pool', lambda e, ws=ws, k=k: e.tensor_copy(out=wob[:, k * D:(k + 1) * D], in_=ws[:]), reads=[ws], writes=[wob])
    W = dict(junk=P.sb("junk", [128, D]), ag=P.sb("ag", [128, 2]), rstd=P.sb("rstd", [128, 1]), tmp=P.sb("tmp", [128, D]))
    hb = [P.sb("hb%d" % i, [128, D]) for i in range(2)]
    ob = [P.sb("ob%d" % i, [128, HK * 128], BF16) for i in range(2)]
    acc = P.sb("acc", [128, D])
    hn = [P.sb("hn%d" % i, [128, D]) for i in range(2)]
    vb = [P.sb("vb%d" % i, [128, D]) for i in range(2)]
    vt32 = P.sb("vt32", [128, D])
    vtb = [P.sb("vtb%d" % i, [128, D], BF16) for i in range(2)]
    lg = P.sb("lg", [128, NEXP])
    m8 = P.sb("m8", [128, 8])
    negm = P.sb("negm", [128, 1])
    msk = P.sb("msk", [128, NEXP])
    ex = P.sb("ex", [128, NEXP])
    ssum = P.sb("ssum", [128, 1])
    gt = [P.sb("gt%d" % i, [128, NEXP]) for i in range(2)]
    py = [P.ps("py%d" % i, [128, 512]) for i in range(2)]
    ptr = P.ps("ptr", [128, D])
    pl = P.ps("pl", [128, NEXP])
    for t in range(NT):
        sfx = 'ctx' if t < CTX // 128 else 'lat'
        h = hb[t % 2]
        o = ob[t % 2]
        if DBG.get('cut', 9) < 1:
            continue
        P.dma('sp', h, h[:], h_d, h_d[t * 128:(t + 1) * 128, :])
        P.dma('sp', o, o[:], oT_d, oT_d[t, :, :])
        if DBG.get('cut', 9) < 2:
            continue
        for nh in range(2):
            for k in range(HK):
                P.mm(py[nh], py[nh][:, :], o, o[:, k * 128:(k + 1) * 128], wob, wob[:, k * D + nh * 512:k * D + (nh + 1) * 512],
                     k == 0, k == HK - 1)
        if DBG.get('cut', 9) < 3:
            continue
        m2 = mb['m2_' + sfx]
        for nh in range(2):
            P.op('dve', lambda e, nh=nh, m2=m2: e.tensor_tensor(out=acc[:, nh * 512:(nh + 1) * 512], in0=py[nh][:, :],
                                                                in1=m2[:, nh * 512:(nh + 1) * 512], op=ALU.mult),
                 reads=[py[nh], m2], writes=[acc])
        P.op('dve', lambda e, h=h: e.scalar_tensor_tensor(out=acc[:], in0=h[:], scalar=ALPHA, in1=acc[:], op0=ALU.mult, op1=ALU.add),
             reads=[h, acc], writes=[acc])
        if DBG.get('cut', 9) < 4:
            continue
        h1 = hn[t % 2]
        v = vb[t % 2]
        _ln_mod(P, W, acc, mb['lnw'], mb['lnb'], mb['m4_' + sfx], mb['m3_' + sfx], h1, v)
        P.dma('sp', h1_d, h1_d[t * 128:(t + 1) * 128, :], h1, h1[:], disjoint=True)
        if DBG.get('cut', 9) < 5:
            continue
        for k in range(KC):
            P.tr(ptr, ptr[:, k * 128:(k + 1) * 128], v, v[:, k * 128:(k + 1) * 128], ident, ident[:])
        P.op('act', lambda e: e.activation(out=vt32[:], in_=ptr[:], func=AF.Copy), reads=[ptr], writes=[vt32])
        vt = vtb[t % 2]
        P.op('dve', lambda e, vt=vt: e.tensor_copy(out=vt[:], in_=vt32[:]), reads=[vt32], writes=[vt])
        P.dma('sp', vT_d, vT_d[t, :, :], vt, vt[:], disjoint=True)
        if DBG.get('norouter'):
            continue
        for k in range(KC):
            P.mm(pl, pl[:, :], vt32, vt32[:, k * 128:(k + 1) * 128], rws, rws[:, k * NEXP:(k + 1) * NEXP], k == 0, False)
        P.mm(pl, pl[:, :], ones, ones[:, :], rbs, rbs[:, :], False, True)
        P.op('dve', lambda e: e.tensor_copy(out=lg[:], in_=pl[:]), reads=[pl], writes=[lg])
        P.op('dve', lambda e: e.max(out=m8[:], in_=lg[:]), reads=[lg], writes=[m8])
        P.op('dve', lambda e: e.tensor_scalar(out=msk[:], in0=lg[:], scalar1=m8[:, 3:4], scalar2=None, op0=ALU.is_ge),
             reads=[lg, m8], writes=[msk])
        P.op('dve', lambda e: e.tensor_scalar(out=negm[:], in0=m8[:, 0:1], scalar1=-1.0, scalar2=None, op0=ALU.mult),
             reads=[m8], writes=[negm])
        P.op('act', lambda e: e.activation(out=ex[:], in_=lg[:], func=AF.Exp, bias=negm[:, 0:1], scale=1.0),
             reads=[lg, negm], writes=[ex])
        P.op('dve', lambda e: e.tensor_tensor(out=ex[:], in0=ex[:], in1=msk[:], op=ALU.mult), reads=[ex, msk], writes=[ex])
        P.op('dve', lambda e: e.reduce_sum(out=ssum[:], in_=ex[:], axis=AX.X), reads=[ex], writes=[ssum])
        P.op('dve', lambda e: e.reciprocal(out=ssum[:], in_=ssum[:]), reads=[ssum], writes=[ssum])
        g = gt[t % 2]
        P.op('dve', lambda e, g=g: e.tensor_scalar(out=g[:], in0=ex[:], scalar1=ssum[:, 0:1], scalar2=None, op0=ALU.mult),
             reads=[ex, ssum], writes=[g])
        P.dma('sp', g_d, g_d[t * 128:(t + 1) * 128, :], g, g[:], disjoint=True)
    rw_np = np.ascontiguousarray(rw.reshape(KC, 128, NEXP).transpose(1, 0, 2).reshape(128, KC * NEXP))
    modb = np.concatenate([_bc(mod[nm]) for nm in names], 1)
    w_np = np.ascontiguousarray(wout.reshape(HK, 128, D), dtype=np.float32)
    in_maps = []
    for c in range(NCORES):
        in_maps.append({"h": np.ascontiguousarray(h_cores[c], dtype=np.float32), "oT": np.ascontiguousarray(oT_cores[c]),
                        "wout": w_np, "modb": modb, "rw": rw_np, "rb": np.ascontiguousarray(rb.reshape(1, NEXP)),
                        "ident": np.eye(128, dtype=np.float32)})
    res = P.run(in_maps)
    return ([res[c]["h1"] for c in range(NCORES)], [res[c]["vT"] for c in range(NCORES)],
            [res[c]["gates"] for c in range(NCORES)])


SWL = 7.0
SWA = 1.702


def launch_moe(vT_all, gates_all, wgu, bgu_in, wdn, bdn, TT):
    P = Prog()
    MP = 'pool' if DBG.get('moe_pool') else 'dve'
    NTT = TT // 128
    vT_d = P.din("vT", [D, TT], BF16)
    g_d = P.din("g", [TT, EPC])
    gT_d = P.din("gT", [EPC, TT])
    wgu_d = P.din("wgu", [EPC * KC, 128, 2 * D])
    wdn_d = P.din("wdn", [EPC * KC, 128, D])
    bgu_d = P.din("bgu", [128, EPC * 16])
    bdn_d = P.din("bdn", [EPC, D])
    out_d = P.dout("part", [TT, D], BF16)
    wgu_s = P.dscratch("wgu_b", [EPC * KC, 128, 2 * D], BF16)
    wdn_s = P.dscratch("wdn_b", [EPC * KC, 128, D], BF16)
    bgu = P.sb("bgu_s", [128, EPC * 16])
    P.dma('sp', bgu, bgu[:], bgu_d, bgu_d[:, :])
    bdn_t = P.sb("bdn_s", [EPC, D])
    P.dma('sp', bdn_t, bdn_t[:], bdn_d, bdn_d[:, :])
    st32 = [P.sb("st32_%d" % i, [128, D]) for i in range(2)]
    st16 = [P.sb("st16_%d" % i, [128, D], BF16) for i in range(2)]
    n = 0
    for i in range(EPC * KC):
        for (src_d, dst_s, c0) in ((wgu_d, wgu_s, 0), (wgu_d, wgu_s, D), (wdn_d, wdn_s, 0)):
            a, b = st32[n % 2], st16[n % 2]
            P.dma('sp', a, a[:, :], src_d, src_d[i, :, c0:c0 + D])
            if n % 2 == 0:
                P.op('act', lambda e, a=a, b=b: e.activation(out=b[:, :], in_=a[:, :], func=AF.Copy), reads=[a], writes=[b])
            else:
                P.op('dve', lambda e, a=a, b=b: e.tensor_copy(out=b[:, :], in_=a[:, :]), reads=[a], writes=[b])
            P.dma('sp', dst_s, dst_s[i, :, c0:c0 + D], b, b[:, :], disjoint=True)
            n += 1
    SBT = 1024
    wg = [P.sb("wg%d" % i, [128, KC * 2 * D], BF16) for i in range(2)]
    wd = [P.sb("wd%d" % i, [128, KC * D], BF16) for i in range(2)]
    vts = P.sb("vts", [128, KC * SBT], BF16)
    acc = [P.sb("acc%d" % i, [128, D]) for i in range(SBT // 128)]
    gsb = P.sb("gsb", [128, (SBT // 128) * EPC])
    accb = [P.sb("accb%d" % i, [128, D], BF16) for i in range(2)]
    gTs = P.sb("gTs", [EPC, SBT])
    actT = [P.sb("actT%d" % i, [128, KC * 512], BF16) for i in range(2)]
    g1 = [P.sb("g1_%d" % i, [128, 512]) for i in range(2)]
    sg = [P.sb("sg_%d" % i, [128, 512]) for i in range(2)]
    u1 = [P.sb("u1_%d" % i, [128, 512]) for i in range(2)]
    u2 = u1
    gl = sg
    pg = [P.ps("pg%d" % i, [128, 512]) for i in range(2)]
    pu = [P.ps("pu%d" % i, [128, 512]) for i in range(2)]
    pd = [P.ps("pd%d" % i, [128, 512]) for i in range(4)]
    wn = 0
    cn = 0
    dn = 0
    for s0 in range(0, TT, SBT):
        sl = min(SBT, TT - s0)
        nt_sb = sl // 128
        P.dma('sp', vts, vts[:, 0:KC * sl].rearrange("p (k t) -> p k t", k=KC), vT_d,
              vT_d[:, s0:s0 + sl].rearrange("(k p) t -> p k t", p=128))
        P.dma('sp', gsb, gsb[:, 0:nt_sb * EPC].rearrange("p (t e) -> p t e", e=EPC), g_d,
              g_d[s0:s0 + sl, :].rearrange("(t p) e -> p t e", p=128))
        P.dma('sp', gTs, gTs[:, 0:sl], gT_d, gT_d[:, s0:s0 + sl])
        for ti in range(nt_sb):
            for nh in range(2):
                pt = pd[dn % 4]
                dn += 1
                P.mm(pt, pt[:, :], gTs, gTs[:, ti * 128:(ti + 1) * 128], bdn_t, bdn_t[:, nh * 512:(nh + 1) * 512], True, True)
                P.op('act', lambda e, ti=ti, nh=nh, pt=pt: e.activation(out=acc[ti][:, nh * 512:(nh + 1) * 512], in_=pt[:, :], func=AF.Copy),
                     reads=[pt], writes=[acc[ti]])
        for e_ in range(EPC):
            wgt, wdt = wg[wn % 2], wd[wn % 2]
            wn += 1
            P.dma('sp', wgt, wgt[:].rearrange("p (k n) -> p k n", k=KC), wgu_s,
                  wgu_s[e_ * KC:(e_ + 1) * KC, :, :].rearrange("k p n -> p k n"))
            P.dma('sp', wdt, wdt[:].rearrange("p (k n) -> p k n", k=KC), wdn_s,
                  wdn_s[e_ * KC:(e_ + 1) * KC, :, :].rearrange("k p n -> p k n"))
            for b0 in range(0, sl, 512):
                nb = min(512, sl - b0)
                at = actT[cn % 2]
                for j in range(KC):
                    q = cn % 2
                    cn += 1
                    for k in range(KC):
                        P.mm(pg[q], pg[q][:, 0:nb], wgt, wgt[:, k * 2 * D + j * 128:k * 2 * D + (j + 1) * 128],
                             vts, vts[:, k * sl + b0:k * sl + b0 + nb], k == 0, k == KC - 1)
                    for k in range(KC):
                        P.mm(pu[q], pu[q][:, 0:nb], wgt, wgt[:, k * 2 * D + D + j * 128:k * 2 * D + D + (j + 1) * 128],
                             vts, vts[:, k * sl + b0:k * sl + b0 + nb], k == 0, k == KC - 1)
                    cg = e_ * 16 + j
                    cu = e_ * 16 + 8 + j
                    P.op('dve', lambda e, q=q, nb=nb, cg=cg: e.tensor_scalar(out=g1[q][:, 0:nb], in0=pg[q][:, 0:nb], scalar1=bgu[:, cg:cg + 1],
                                                                          scalar2=SWL, op0=ALU.add, op1=ALU.min), reads=[pg[q], bgu], writes=[g1[q]])
                    P.op('act', lambda e, q=q, nb=nb: e.activation(out=sg[q][:, 0:nb], in_=g1[q][:, 0:nb], func=AF.Sigmoid, scale=SWA),
                         reads=[g1[q]], writes=[sg[q]])
                    P.op('dve', lambda e, q=q, nb=nb, cu=cu: e.tensor_scalar(out=u1[q][:, 0:nb], in0=pu[q][:, 0:nb], scalar1=bgu[:, cu:cu + 1],
                                                                          scalar2=SWL, op0=ALU.add, op1=ALU.min), reads=[pu[q], bgu], writes=[u1[q]])
                    P.op(MP, lambda e, q=q, nb=nb: e.tensor_scalar(out=u2[q][:, 0:nb], in0=u1[q][:, 0:nb], scalar1=-SWL, scalar2=1.0,
                                                                       op0=ALU.max, op1=ALU.add), reads=[u1[q]], writes=[u2[q]])
                    P.op(MP, lambda e, q=q, nb=nb: e.tensor_tensor(out=gl[q][:, 0:nb], in0=g1[q][:, 0:nb], in1=sg[q][:, 0:nb], op=ALU.mult),
                         reads=[g1[q], sg[q]], writes=[gl[q]])
                    P.op('dve', lambda e, q=q, nb=nb, j=j, at=at: e.tensor_tensor(out=at[:, j * 512:j * 512 + nb], in0=gl[q][:, 0:nb], in1=u2[q][:, 0:nb], op=ALU.mult),
                         reads=[gl[q], u2[q]], writes=[at])
                for ti in range(nb // 128):
                    tg = (b0 // 128) + ti
                    for nh in range(2):
                        pt = pd[dn % 4]
                        dn += 1
                        for j in range(KC):
                            P.mm(pt, pt[:, :], at, at[:, j * 512 + ti * 128:j * 512 + (ti + 1) * 128], wdt,
                                 wdt[:, j * D + nh * 512:j * D + (nh + 1) * 512], j == 0, j == KC - 1)
                        gc = tg * EPC + e_
                        P.op('dve', lambda e, pt=pt, tg=tg, nh=nh, gc=gc: e.scalar_tensor_tensor(
                            out=acc[tg][:, nh * 512:(nh + 1) * 512], in0=pt[:, :], scalar=gsb[:, gc:gc + 1],
                            in1=acc[tg][:, nh * 512:(nh + 1) * 512], op0=ALU.mult, op1=ALU.add), reads=[pt, gsb, acc[tg]], writes=[acc[tg]])
        for ti in range(nt_sb):
            ab_ = accb[ti % 2]
            P.op('act', lambda e, ab_=ab_, ti=ti: e.activation(out=ab_[:], in_=acc[ti][:], func=AF.Copy), reads=[acc[ti]], writes=[ab_])
            P.dma('sp', out_d, out_d[s0 + ti * 128:s0 + (ti + 1) * 128, :], ab_, ab_[:], disjoint=True)
    in_maps = []
    for c in range(NCORES):
        es = slice(c * EPC, (c + 1) * EPC)
        in_maps.append({
            "vT": np.ascontiguousarray(vT_all), "g": np.ascontiguousarray(gates_all[:, es]),
            "gT": np.ascontiguousarray(gates_all[:, es].T),
            "wgu": np.ascontiguousarray(wgu[es].reshape(EPC * KC, 128, 2 * D)),
            "wdn": np.ascontiguousarray(wdn[es].reshape(EPC * KC, 128, D)),
            "bgu": np.ascontiguousarray(bgu_np_layout(bgu_in[es])),
            "bdn": np.ascontiguousarray(bdn[es])})
    res = P.run(in_maps)
    return [res[c]["part"] for c in range(NCORES)]


def bgu_np(bgu, es):
    return bgu[es]


def bgu_np_layout(bgu_all):
    return bgu_all.reshape(EPC, 16, 128).transpose(2, 0, 1).reshape(128, EPC * 16)


def rope_tables(L_lat):
    GRID_W = 64
    pos = np.arange(L_lat)
    row = (pos // GRID_W).astype(np.float32)
    col = (pos % GRID_W).astype(np.float32)
    axis_dim = 32
    inv = (10000.0 ** (-np.arange(0, axis_dim, 2, dtype=np.float32) / axis_dim)).astype(np.float32)
    ar = row[:, None] * inv
    ac = col[:, None] * inv
    ang = np.concatenate([ar, ar, ac, ac], -1)
    cos = np.cos(ang).astype(np.float32)
    sin = np.sin(ang).astype(np.float32)
    sign = np.concatenate([-np.ones(16), np.ones(16), -np.ones(16), np.ones(16)]).astype(np.float32)
    cosT = np.concatenate([np.ones((64, CTX), np.float32), cos.T], 1)
    sinT = np.concatenate([np.zeros((64, CTX), np.float32), (sin * sign).T], 1)
    return np.ascontiguousarray(cosT), np.ascontiguousarray(sinT)


ROT_PERM = np.concatenate([np.arange(16, 32), np.arange(0, 16), np.arange(48, 64), np.arange(32, 48)])


def launch_attn(uT_all, w_in, lam, subln_w, lambda_init, TT):
    P = Prog()
    NTT = TT // 128
    L = TT - CTX
    uT_d = P.din("uT", [D, TT], BF16)
    w_d = P.din("w", [5 * KC, 128, 128])
    cos_d = P.din("cos", [64, TT])
    sin_d = P.din("sin", [64, TT])
    lam_d = P.din("lam", [128, 256])
    sw_d = P.din("sw", [128, 1])
    sel_d = P.din("sel", [128, 256], BF16)
    oT_d = P.dout("oT", [128, TT], BF16)
    sel = P.sb("sel_s", [128, 256], BF16)
    P.dma('sp', sel, sel[:], sel_d, sel_d[:, :])
    ones_b = P.sb("ones_b", [128, 128], BF16)
    ones_f = P.sb("ones_f", [128, 128])
    P.op('dve', lambda e: e.memset(ones_b[:], 1.0), writes=[ones_b])
    P.op('dve', lambda e: e.memset(ones_f[:], 1.0), writes=[ones_f])
    lamt = P.sb("lamt", [128, 256])
    P.dma('sp', lamt, lamt[:], lam_d, lam_d[:, :])
    sw = P.sb("sw_s", [128, 1])
    P.dma('sp', sw, sw[:], sw_d, sw_d[:, :])
    P.op('dve', lambda e: e.tensor_scalar(out=sw[:], in0=sw[:], scalar1=1.0 - lambda_init, scalar2=None, op0=ALU.mult), reads=[sw], writes=[sw])
    lp = P.sb("lp", [128, 128])
    ls = P.sb("ls", [128, 2])
    neglam = P.sb("neglam", [128, 1])
    P.op('dve', lambda e: e.tensor_tensor(out=lp[:, 0:64], in0=lamt[:, 0:64], in1=lamt[:, 64:128], op=ALU.mult), reads=[lamt], writes=[lp])
    P.op('dve', lambda e: e.tensor_tensor(out=lp[:, 64:128], in0=lamt[:, 128:192], in1=lamt[:, 192:256], op=ALU.mult), reads=[lamt], writes=[lp])
    P.op('dve', lambda e: e.reduce_sum(out=ls[:, 0:1], in_=lp[:, 0:64], axis=AX.X), reads=[lp], writes=[ls])
    P.op('dve', lambda e: e.reduce_sum(out=ls[:, 1:2], in_=lp[:, 64:128], axis=AX.X), reads=[lp, ls], writes=[ls])
    P.op('act', lambda e: e.activation(out=ls[:], in_=ls[:], func=AF.Exp), reads=[ls], writes=[ls])
    P.op('dve', lambda e: e.tensor_tensor(out=neglam[:], in0=ls[:, 1:2], in1=ls[:, 0:1], op=ALU.subtract), reads=[ls], writes=[neglam])
    P.op('dve', lambda e: e.tensor_scalar(out=neglam[:], in0=neglam[:], scalar1=-lambda_init, scalar2=None, op0=ALU.add), reads=[neglam], writes=[neglam])
    wst = [P.sb("wst%d" % i, [128, 128]) for i in range(2)]
    wb = P.sb("wb", [128, 5 * KC * 128], BF16)
    for i in range(5 * KC):
        a = wst[i % 2]
        P.dma('sp', a, a[:], w_d, w_d[i, :, :])
        P.op('act', lambda e, a=a, i=i: e.activation(out=wb[:, i * 128:(i + 1) * 128], in_=a[:], func=AF.Copy), reads=[a], writes=[wb])
    QT = P.sb("QT", [128, TT], BF16)
    KT = P.sb("KT", [128, TT], BF16)
    Vs = P.sb("Vs", [128, NTT * 128], BF16)
    ub = [P.sb("ub%d" % i, [128, KC * 512], BF16) for i in range(2)]
    cs = [P.sb("cs%d" % i, [128, 512]) for i in range(2)]
    sn = [P.sb("sn%d" % i, [128, 512]) for i in range(2)]
    x1 = P.sb("x1", [128, 512])
    x2 = P.sb("x2", [128, 512])
    sq = P.sb("sq", [128, 512], BF16)
    mx = P.sb("mx", [128, 4])
    mt = P.sb("mt", [128, 1])
    P.op('dve', lambda e: e.memset(mx[:], 0.0), writes=[mx])
    pA = [P.ps("pA%d" % i, [128, 512]) for i in range(2)]
    pO = [P.ps("pO%d" % i, [128, 512]) for i in range(2)]
    pL = [P.ps("pL%d" % i, [128, 512]) for i in range(2)]
    pN = P.ps("pN", [128, 512])
    for bi, b0 in enumerate(range(0, TT, 512)):
        nb = min(512, TT - b0)
        u = ub[bi % 2]
        c_, s_ = cs[bi % 2], sn[bi % 2]
        P.dma('sp', u, u[:, 0:KC * nb].rearrange("p (k t) -> p k t", k=KC), uT_d, uT_d[:, b0:b0 + nb].rearrange("(k p) t -> p k t", p=128))
        for hh in range(2):
            P.dma('sp', c_, c_[hh * 64:(hh + 1) * 64, 0:nb], cos_d, cos_d[:, b0:b0 + nb])
            P.dma('sp', s_, s_[hh * 64:(hh + 1) * 64, 0:nb], sin_d, sin_d[:, b0:b0 + nb])
        for xi, XT in enumerate((QT, KT)):
            for k in range(KC):
                P.mm(pA[0], pA[0][:, 0:nb], wb, wb[:, (2 * xi * KC + k) * 128:(2 * xi * KC + k + 1) * 128], u, u[:, k * nb:(k + 1) * nb], k == 0, k == KC - 1)
            for k in range(KC):
                P.mm(pA[1], pA[1][:, 0:nb], wb, wb[:, ((2 * xi + 1) * KC + k) * 128:((2 * xi + 1) * KC + k + 1) * 128], u, u[:, k * nb:(k + 1) * nb], k == 0, k == KC - 1)
            P.op('dve', lambda e, nb=nb, c_=c_: e.tensor_tensor(out=x1[:, 0:nb], in0=pA[0][:, 0:nb], in1=c_[:, 0:nb], op=ALU.mult), reads=[pA[0], c_], writes=[x1])
            P.op('dve', lambda e, nb=nb, s_=s_: e.tensor_tensor(out=x2[:, 0:nb], in0=pA[1][:, 0:nb], in1=s_[:, 0:nb], op=ALU.mult), reads=[pA[1], s_], writes=[x2])
            P.op('dve', lambda e, nb=nb, XT=XT, b0=b0: e.tensor_tensor(out=XT[:, b0:b0 + nb], in0=x1[:, 0:nb], in1=x2[:, 0:nb], op=ALU.add), reads=[x1, x2], writes=[XT])
            P.op('act', lambda e, nb=nb, XT=XT, b0=b0: e.activation(out=sq[:, 0:nb], in_=XT[:, b0:b0 + nb], func=AF.Square), reads=[XT], writes=[sq])
            for m in range(2):
                P.mm(pN, pN[:, 0:nb], sel, sel[:, m * 128:(m + 1) * 128], sq, sq[:, 0:nb], True, True)
                P.op('dve', lambda e, nb=nb: e.reduce_max(out=mt[:], in_=pN[:, 0:nb], axis=AX.X), reads=[pN], writes=[mt])
                col = xi * 2 + m
                P.op('dve', lambda e, col=col: e.tensor_tensor(out=mx[:, col:col + 1], in0=mx[:, col:col + 1], in1=mt[:], op=ALU.max), reads=[mx, mt], writes=[mx])
        for ti in range(nb // 128):
            tg = b0 // 128 + ti
            pv = pO[ti % 2]
            for k in range(KC):
                P.mm(pv, pv[:, 0:128], u, u[:, k * nb + ti * 128:k * nb + (ti + 1) * 128], wb, wb[:, (4 * KC + k) * 128:(4 * KC + k + 1) * 128], k == 0, k == KC - 1)
            P.op('act', lambda e, pv=pv, tg=tg: e.activation(out=Vs[:, tg * 128:(tg + 1) * 128], in_=pv[:, 0:128], func=AF.Copy), reads=[pv], writes=[Vs])
    negc = P.sb("negc", [128, 2])
    P.op('dve', lambda e: e.tensor_tensor(out=negc[:], in0=mx[:, 0:2], in1=mx[:, 2:4], op=ALU.mult), reads=[mx], writes=[negc])
    P.op('act', lambda e: e.activation(out=negc[:], in_=negc[:], func=AF.Sqrt), reads=[negc], writes=[negc])
    P.op('dve', lambda e: e.tensor_scalar(out=negc[:], in0=negc[:], scalar1=-1.05 * 0.125, scalar2=None, op0=ALU.mult), reads=[negc], writes=[negc])
    Pt = [P.sb("Pt%d" % i, [128, 512], BF16) for i in range(3)]
    r0 = P.sb("r0", [128, 512])
    r1 = P.sb("r1", [128, 512])
    o0 = P.sb("o0", [128, 512])
    o1 = P.sb("o1", [128, 512])
    a_ = P.sb("a_", [128, 512])
    sqf = P.sb("sqf", [128, 512])
    rs = P.sb("rs", [128, 512])
    ob = [P.sb("ob%d" % i, [128, 512], BF16) for i in range(2)]
    pS = [pA[0], pA[1]]
    qblocks = [(0, CTX, CTX // 128)] + [(q0, min(512, TT - q0), NTT) for q0 in range(CTX, TT, 512)]
    it = 0
    NPS = len(pS)
    for qi, (q0, nq, nkt) in enumerate(qblocks):
        steps = [(m, kt) for m in range(2) for kt in range(nkt)]

        def score(i, it_):
            m, kt = steps[i]
            rows = slice(m * 64, (m + 1) * 64)
            ps = pS[it_ % NPS]
            P.mm(ps, ps[:, 0:nq], KT, KT[rows, kt * 128:(kt + 1) * 128], QT, QT[rows, q0:q0 + nq], True, True)
        score(0, it)
        for i, (m, kt) in enumerate(steps):
            ps = pS[it % NPS]
            pt = Pt[it % 3]
            if i + 1 < len(steps):
                score(i + 1, it + 1)
            P.op('act', lambda e, ps=ps, pt=pt, nq=nq, m=m: e.activation(out=pt[:, 0:nq], in_=ps[:, 0:nq], func=AF.Exp, bias=negc[:, m:m + 1], scale=0.125),
                 reads=[ps, negc], writes=[pt])
            P.mm(pO[m], pO[m][:, 0:nq], Vs, Vs[:, kt * 128:(kt + 1) * 128], pt, pt[:, 0:nq], kt == 0, kt == nkt - 1)
            P.mm(pL[m], pL[m][:, 0:nq], ones_b, ones_b[:, :], pt, pt[:, 0:nq], kt == 0, kt == nkt - 1)
            it += 1
        P.op('dve', lambda e, nq=nq: e.reciprocal(out=r0[:, 0:nq], in_=pL[0][:, 0:nq]), reads=[pL[0]], writes=[r0])
        P.op('dve', lambda e, nq=nq: e.reciprocal(out=r1[:, 0:nq], in_=pL[1][:, 0:nq]), reads=[pL[1]], writes=[r1])
        P.op('dve', lambda e, nq=nq: e.tensor_tensor(out=o0[:, 0:nq], in0=pO[0][:, 0:nq], in1=r0[:, 0:nq], op=ALU.mult), reads=[pO[0], r0], writes=[o0])
        P.op('dve', lambda e, nq=nq: e.tensor_tensor(out=o1[:, 0:nq], in0=pO[1][:, 0:nq], in1=r1[:, 0:nq], op=ALU.mult), reads=[pO[1], r1], writes=[o1])
        P.op('dve', lambda e, nq=nq: e.scalar_tensor_tensor(out=a_[:, 0:nq], in0=o1[:, 0:nq], scalar=neglam[:, 0:1], in1=o0[:, 0:nq], op0=ALU.mult, op1=ALU.add),
             reads=[o0, o1, neglam], writes=[a_])
        P.op('act', lambda e, nq=nq: e.activation(out=sqf[:, 0:nq], in_=a_[:, 0:nq], func=AF.Square), reads=[a_], writes=[sqf])
        P.mm(pN, pN[:, 0:nq], ones_f, ones_f[:, :], sqf, sqf[:, 0:nq], True, True)
        P.op('act', lambda e, nq=nq: e.activation(out=rs[:, 0:nq], in_=pN[:, 0:nq], func=AF.Sqrt, bias=EPS, scale=1.0 / 128), reads=[pN], writes=[rs])
        P.op('dve', lambda e, nq=nq: e.reciprocal(out=rs[:, 0:nq], in_=rs[:, 0:nq]), reads=[rs], writes=[rs])
        P.op('dve', lambda e, nq=nq: e.tensor_tensor(out=a_[:, 0:nq], in0=a_[:, 0:nq], in1=rs[:, 0:nq], op=ALU.mult), reads=[a_, rs], writes=[a_])
        o_ = ob[qi % 2]
        P.op('dve', lambda e, nq=nq, o_=o_: e.tensor_scalar(out=o_[:, 0:nq], in0=a_[:, 0:nq], scalar1=sw[:, 0:1], scalar2=None, op0=ALU.mult), reads=[a_, sw], writes=[o_])
        P.dma('sp', oT_d, oT_d[:, q0:q0 + nq], o_, o_[:, 0:nq], disjoint=True)
    cosT, sinT = rope_tables(L)
    selnp = np.zeros((128, 256), np.float32)
    selnp[0:64, 0:128] = 1.0
    selnp[64:128, 128:256] = 1.0
    in_maps = []
    for c in range(NCORES):
        cols = []
        for base in (0, D):
            wc = w_in[:, base + c * 128:base + (c + 1) * 128]
            wp = np.concatenate([wc[:, 0:64][:, ROT_PERM], wc[:, 64:128][:, ROT_PERM]], 1)
            cols += [wc, wp]
        cols.append(w_in[:, 2 * D + c * 128:2 * D + (c + 1) * 128])
        wnp = np.stack([x.reshape(KC, 128, 128) for x in cols], 0).reshape(5 * KC, 128, 128)
        in_maps.append({"uT": np.ascontiguousarray(uT_all), "w": np.ascontiguousarray(wnp, dtype=np.float32),
                        "cos": cosT, "sin": sinT, "lam": _bc(lam.reshape(-1)), "sw": np.ascontiguousarray(subln_w.reshape(128, 1), dtype=np.float32),
                        "sel": selnp.astype(NPBF)})
    res = P.run(in_maps)
    return np.concatenate([res[c]["oT"] for c in range(NCORES)], 0)


def gdn_consts():
    i = np.arange(128)
    same = (i[:, None] // 64) == (i[None, :] // 64)
    le = i[:, None] <= i[None, :]
    ge = i[:, None] >= i[None, :]
    lt = i[:, None] < i[None, :]
    gt = i[:, None] > i[None, :]
    mats = []
    for d in range(2):
        mcum = same & (le if d == 0 else ge)
        maskS = same & (gt if d == 0 else lt)
        maskIT = same & (le if d == 0 else ge)
        mats += [mcum, maskS, maskIT]
    mats += [same, np.eye(128, dtype=bool)]
    c = np.concatenate([m.astype(np.float32) for m in mats], 1)
    cind = np.stack([(i < 64), (i >= 64)], 1).astype(np.float32)
    return np.ascontiguousarray(c), cind


def launch_gdn(uT_all, w_in, conv_w, a_log, dt_bias, norm_w, TT):
    P = Prog()
    NTT = TT // 128
    RSQ = 128 ** -0.5
    GW = 516
    uT_d = P.din("uT", [D, TT], BF16)
    w_d = P.din("w", [4 * KC, 128, 128])
    wz_d = P.din("wz", [KC, 128, 256])
    wab_d = P.din("wab", [KC, 128, 8])
    cw_d = P.din("cw", [128, 20])
    cst_d = P.din("cst", [128, 8 * 128])
    cind_d = P.din("cind", [128, 2])
    alog_d = P.din("alog", [128, 4])
    dtb_d = P.din("dtb", [128, 4])
    nw_d = P.din("nw", [128, 128])
    oT_d = P.dout("oT", [256, TT], BF16)
    of_s = P.dscratch("of_s", [TT, 256])
    dbg_d = P.dout("dbg", [TT, 1024]) if DBG.get("gdbg") else None

    def D_(fn, r, w):
        P.op('dve', fn, reads=r, writes=w)

    def A_(fn, r, w):
        P.op('act', fn, reads=r, writes=w)

    def G_(fn, r, w):
        P.op('pool', fn, reads=r, writes=w)

    def load(name, shape, src_d, dt=F32):
        b = P.sb(name, shape, dt)
        P.dma('sp', b, b[:], src_d, src_d[tuple(slice(None) for _ in shape)])
        return b
    cst = load("cst_s", [128, 8 * 128], cst_d)
    cind = load("cind_s", [128, 2], cind_d)
    cw = load("cw_s", [128, 20], cw_d)
    negA = load("negA", [128, 4], alog_d)
    dtb = load("dtb_s", [128, 4], dtb_d)
    nwb = load("nwb", [128, 128], nw_d)
    A_(lambda e: e.activation(out=negA[:], in_=negA[:], func=AF.Exp), [negA], [negA])
    D_(lambda e: e.tensor_scalar(out=negA[:], in0=negA[:], scalar1=-1.0, scalar2=None, op0=ALU.mult), [negA], [negA])

    def C(idx):
        return cst[:, idx * 128:(idx + 1) * 128]
    SAME, IDENT = 6, 7
    ones_f = P.sb("ones_f", [128, 128])
    negones = P.sb("negones", [128, 128])
    D_(lambda e: e.memset(ones_f[:], 1.0), [], [ones_f])
    D_(lambda e: e.memset(negones[:], -1.0), [], [negones])
    wst = [P.sb("wst%d" % i, [128, 256]) for i in range(2)]
    wb = P.sb("wb", [128, 4 * KC * 128], BF16)
    wzb = P.sb("wzb", [128, KC * 256], BF16)
    wabb = P.sb("wabb", [128, KC * 8], BF16)
    n = 0
    for i in range(4 * KC):
        a = wst[n % 2]; n += 1
        P.dma('sp', a, a[:, 0:128], w_d, w_d[i, :, :])
        A_(lambda e, a=a, i=i: e.activation(out=wb[:, i * 128:(i + 1) * 128], in_=a[:, 0:128], func=AF.Copy), [a], [wb])
    for k in range(KC):
        a = wst[n % 2]; n += 1
        P.dma('sp', a, a[:, 0:256], wz_d, wz_d[k, :, :])
        A_(lambda e, a=a, k=k: e.activation(out=wzb[:, k * 256:(k + 1) * 256], in_=a[:, 0:256], func=AF.Copy), [a], [wzb])
    for k in range(KC):
        a = wst[n % 2]; n += 1
        P.dma('sp', a, a[:, 0:8], wab_d, wab_d[k, :, :])
        A_(lambda e, a=a, k=k: e.activation(out=wabb[:, k * 8:(k + 1) * 8], in_=a[:, 0:8], func=AF.Copy), [a], [wabb])
    S = {}
    for d in range(2):
        for hl in range(2):
            S[(d, hl)] = P.sb("S%d%d" % (d, hl), [128, 128])
            D_(lambda e, b=S[(d, hl)]: e.memset(b[:], 0.0), [], [S[(d, hl)]])
    ug = [P.sb("ug%d" % i, [128, KC * GW], BF16) for i in range(2)]
    pA = [P.ps("pA%d" % i, [128, 512]) for i in range(2)]
    pD = P.ps("pD", [128, 512])
    pM = [P.ps("pM%d" % i, [128, 512]) for i in range(2)]
    pU = P.ps("pU", [128, 512])
    pR = P.ps("pR", [128, 512])
    pKV = P.ps("pKV", [128, 512]) if DBG.get("p8") else None
    pKVb = pKV if pKV is not None else pU
    kvo = 0 if pKV is not None else 256
    bufs = {}

    def B(name, par, shape=(128, 128), dt=F32):
        key = (name, par)
        if key not in bufs:
            bufs[key] = P.sb("%s_%d" % (name, par), list(shape), dt)
        return bufs[key]

    ctx_tiles = list(range(CTX // 128))
    lat_tiles = list(range(CTX // 128, NTT))

    def groups(tiles):
        return [tiles[i:i + 4] for i in range(0, len(tiles), 4)]
    order = {0: [(g, 0, CTX) for g in groups(ctx_tiles)] + [(g, CTX, TT) for g in groups(lat_tiles)],
             1: [(g[::-1], 0, CTX) for g in groups(ctx_tiles)[::-1]] + [(g[::-1], CTX, TT) for g in groups(lat_tiles)[::-1]]}
    gi = 0
    tcount = 0
    for d in range(2):
        MC, MS, MIT = 3 * d, 3 * d + 1, 3 * d + 2
        for (gt, seg0, seg1) in order[d]:
            g0 = min(gt) * 128
            g1 = (max(gt) + 1) * 128
            glo = max(g0 - 2, seg0)
            ghi = min(g1 + 2, seg1)
            gw = ghi - glo
            u = ug[gi % 2]
            gi += 1
            P.dma('sp', u, u[:].rearrange("p (k t) -> p k t", k=KC)[:, :, 0:gw], uT_d,
                  uT_d[:, glo:ghi].rearrange("(k p) t -> p k t", p=128))
            for tile in gt:
                par = tcount % 2
                tcount += 1
                t0 = tile * 128
                lo = max(t0 - 2, seg0)
                hi = min(t0 + 130, seg1)
                ncol = hi - lo
                off = lo - (t0 - 2)
                X = []
                for ci in range(4):
                    pp = pA[ci % 2]
                    for k in range(KC):
                        P.mm(pp, pp[:, 0:ncol], wb, wb[:, (ci * KC + k) * 128:(ci * KC + k + 1) * 128],
                             u, u[:, k * GW + lo - glo:k * GW + hi - glo], k == 0, k == KC - 1)
                    pb = B("pbuf%d" % ci, par, (128, 132))
                    D_(lambda e, pb=pb: e.memset(pb[:, 0:2], 0.0), [], [pb])
                    D_(lambda e, pb=pb: e.memset(pb[:, 130:132], 0.0), [], [pb])
                    A_(lambda e, pb=pb, pp=pp, off=off, ncol=ncol: e.activation(out=pb[:, off:off + ncol], in_=pp[:, 0:ncol], func=AF.Copy), [pp], [pb])
                    ca = B("cacc%d" % ci, par)
                    D_(lambda e, ca=ca, pb=pb, ci=ci: e.tensor_scalar(out=ca[:], in0=pb[:, 0:128], scalar1=cw[:, ci * 5:ci * 5 + 1], scalar2=None, op0=ALU.mult),
                       [pb, cw], [ca])
                    for j in range(1, 5):
                        D_(lambda e, ca=ca, pb=pb, ci=ci, j=j: e.scalar_tensor_tensor(out=ca[:], in0=pb[:, j:j + 128], scalar=cw[:, ci * 5 + j:ci * 5 + j + 1],
                                                                                   in1=ca[:], op0=ALU.mult, op1=ALU.add), [pb, cw, ca], [ca])
                    sg_ = B("csg%d" % ci, par)
                    A_(lambda e, sg_=sg_, ca=ca: e.activation(out=sg_[:], in_=ca[:], func=AF.Sigmoid), [ca], [sg_])
                    x = B("X%d" % ci, par)
                    G_(lambda e, x=x, ca=ca, sg_=sg_: e.tensor_tensor(out=x[:], in0=ca[:], in1=sg_[:], op=ALU.mult), [ca, sg_], [x])
                    X.append(x)
                for ci in range(2):
                    x = X[ci]
                    sq = B("sq%d" % ci, par)
                    A_(lambda e, sq=sq, x=x: e.activation(out=sq[:], in_=x[:], func=AF.Square), [x], [sq])
                    pn = pA[ci % 2]
                    P.mm(pn, pn[:, 0:128], ones_f, ones_f[:, :], sq, sq[:, :], True, True)
                    rn = B("rn%d" % ci, par)
                    A_(lambda e, rn=rn, pn=pn: e.activation(out=rn[:], in_=pn[:, 0:128], func=AF.Sqrt, bias=1e-6, scale=1.0), [pn], [rn])
                    D_(lambda e, rn=rn: e.reciprocal(out=rn[:], in_=rn[:]), [rn], [rn])
                    D_(lambda e, x=x, rn=rn: e.tensor_tensor(out=x[:], in0=x[:], in1=rn[:], op=ALU.mult), [x, rn], [x])
                qT, kT = X[0], X[1]
                if DBG.get('gcut', 9) < 1:
                    continue
                tok = []
                for ci in (1, 2, 3):
                    pt = pA[ci % 2]
                    P.tr(pt, pt[:, 0:128], X[ci], X[ci][:, :], cst, C(IDENT))
                    tb = B("tok%d" % ci, par)
                    A_(lambda e, tb=tb, pt=pt: e.activation(out=tb[:], in_=pt[:, 0:128], func=AF.Copy), [pt], [tb])
                    tok.append(tb)
                Kt, V = tok[0], tok[1:]
                if DBG.get('gcut', 9) < 2:
                    continue
                pab = pA[0]
                for k in range(KC):
                    P.mm(pab, pab[:, 0:8], u, u[:, k * GW + t0 - glo:k * GW + t0 - glo + 128], wabb, wabb[:, k * 8:(k + 1) * 8], k == 0, k == KC - 1)
                ab = B("ab", par, (128, 8))
                D_(lambda e, ab=ab, pab=pab: e.tensor_copy(out=ab[:], in_=pab[:, 0:8]), [pab], [ab])
                xa = B("xa", par, (128, 2)); ax = B("ax", par, (128, 2)); l1 = B("l1", par, (128, 2)); g2 = B("g2", par, (128, 2))
                be2 = B("be2", par, (128, 2))
                D_(lambda e, xa=xa, ab=ab, d=d: e.tensor_tensor(out=xa[:], in0=ab[:, d * 2:d * 2 + 2], in1=dtb[:, d * 2:d * 2 + 2], op=ALU.add), [ab, dtb], [xa])
                A_(lambda e, xa=xa, ax=ax: e.activation(out=ax[:], in_=xa[:], func=AF.Abs), [xa], [ax])
                A_(lambda e, ax=ax, l1=l1: e.activation(out=l1[:], in_=ax[:], func=AF.Exp, scale=-1.0), [ax], [l1])
                A_(lambda e, l1=l1: e.activation(out=l1[:], in_=l1[:], func=AF.Ln, bias=1.0, scale=1.0), [l1], [l1])
                D_(lambda e, xa=xa: e.tensor_scalar(out=xa[:], in0=xa[:], scalar1=0.0, scalar2=None, op0=ALU.max), [xa], [xa])
                D_(lambda e, xa=xa, l1=l1: e.tensor_tensor(out=xa[:], in0=xa[:], in1=l1[:], op=ALU.add), [xa, l1], [xa])
                D_(lambda e, xa=xa, g2=g2, d=d: e.tensor_tensor(out=g2[:], in0=xa[:], in1=negA[:, d * 2:d * 2 + 2], op=ALU.mult), [xa, negA], [g2])
                A_(lambda e, be2=be2, ab=ab, d=d: e.activation(out=be2[:], in_=ab[:, 4 + d * 2:4 + d * 2 + 2], func=AF.Sigmoid), [ab], [be2])
                pg = pA[1]
                P.mm(pg, pg[:, 0:2], cst, C(MC), g2, g2[:, :], True, True)
                P.mm(pg, pg[:, 2:4], cst, C(SAME), g2, g2[:, :], True, True)
                gsel = B("gsel", par, (128, 4))
                for cb in range(2):
                    D_(lambda e, gsel=gsel, g2=g2, cb=cb: e.tensor_scalar(out=gsel[:, cb * 2:cb * 2 + 2], in0=cind[:, :], scalar1=g2[:, cb:cb + 1], scalar2=None, op0=ALU.mult),
                       [cind, g2], [gsel])
                P.mm(pg, pg[:, 4:8], ones_f, ones_f[:, :], gsel, gsel[:, :], True, True)
                gcl = B("gcl", par, (128, 4))
                D_(lambda e, gcl=gcl, pg=pg: e.tensor_copy(out=gcl[:], in_=pg[:, 0:4]), [pg], [gcl])
                eglb = B("eglb", par, (128, 4))
                A_(lambda e, eglb=eglb, pg=pg: e.activation(out=eglb[:], in_=pg[:, 4:8], func=AF.Exp), [pg], [eglb])
                egc = B("egc", par, (128, 2)); bk2 = B("bk2", par, (128, 2)); kd2 = B("kd2", par, (128, 2)); qs2 = B("qs2", par, (128, 2))
                A_(lambda e, egc=egc, gcl=gcl: e.activation(out=egc[:], in_=gcl[:, 0:2], func=AF.Exp), [gcl], [egc])
                D_(lambda e, bk2=bk2, be2=be2, egc=egc: e.tensor_tensor(out=bk2[:], in0=be2[:], in1=egc[:], op=ALU.mult), [be2, egc], [bk2])
                D_(lambda e, kd2=kd2, gcl=gcl: e.tensor_tensor(out=kd2[:], in0=gcl[:, 2:4], in1=gcl[:, 0:2], op=ALU.subtract), [gcl], [kd2])
                A_(lambda e, kd2=kd2: e.activation(out=kd2[:], in_=kd2[:], func=AF.Exp), [kd2], [kd2])
                D_(lambda e, qs2=qs2, egc=egc: e.tensor_scalar(out=qs2[:], in0=egc[:], scalar1=RSQ, scalar2=None, op0=ALU.mult), [egc], [qs2])
                if DBG.get('gcut', 9) < 3:
                    continue
                pk = pM[0]
                P.mm(pk, pk[:, 0:128], kT, kT[:, :], kT, kT[:, :], True, True)
                KKm = B("KKm", par)
                D_(lambda e, KKm=KKm, pk=pk, MS=MS: e.tensor_tensor(out=KKm[:], in0=pk[:, 0:128], in1=C(MS), op=ALU.mult), [pk, cst], [KKm])
                pq = pM[1]
                P.mm(pq, pq[:, 0:128], kT, kT[:, :], qT, qT[:, :], True, True)
                QKTm = B("QKTm", par)
                D_(lambda e, QKTm=QKTm, pq=pq, MIT=MIT: e.scalar_tensor_tensor(out=QKTm[:], in0=pq[:, 0:128], scalar=RSQ, in1=C(MIT), op0=ALU.mult, op1=ALU.mult),
                   [pq, cst], [QKTm])
                if DBG.get('gcut', 9) < 4 or (DBG.get('gcut', 9) == 4 and DBG.get('sub', 0) == 0):
                    continue
                o_t = B("o_t", par, (128, 256))
                for cb in range(2):
                    sfx = "%d" % cb
                    gM = B("gM" + sfx, par)
                    D_(lambda e, gM=gM, g2=g2, cb=cb, MC=MC: e.tensor_scalar(out=gM[:], in0=C(MC), scalar1=g2[:, cb:cb + 1], scalar2=None, op0=ALU.mult), [cst, g2], [gM])
                    P.mm(pD, pD[:, 0:128], gM, gM[:, :], ones_f, ones_f[:, :], True, False)
                    P.mm(pD, pD[:, 0:128], negones, negones[:, :], gM, gM[:, :], False, True)
                    if DBG.get('gcut', 9) == 4 and DBG.get('sub', 0) == 1:
                        continue
                    E1 = B("E1" + sfx, par); E2 = B("E2" + sfx, par)
                    D_(lambda e, E1=E1: e.tensor_scalar(out=E1[:], in0=pD[:, 0:128], scalar1=0.0, scalar2=None, op0=ALU.min), [pD], [E1])
                    D_(lambda e, E2=E2: e.tensor_scalar(out=E2[:], in0=pD[:, 0:128], scalar1=0.0, scalar2=None, op0=ALU.max), [pD], [E2])
                    if DBG.get('gcut', 9) == 4 and DBG.get('sub', 0) == 2:
                        continue
                    A_(lambda e, E1=E1: e.activation(out=E1[:], in_=E1[:], func=AF.Exp), [E1], [E1])
                    A_(lambda e, E2=E2: e.activation(out=E2[:], in_=E2[:], func=AF.Exp, scale=-1.0), [E2], [E2])
                    if DBG.get('gcut', 9) < 5 and DBG.get('sub', 0) == 3:
                        continue
                    if DBG.get('gcut', 9) < 5:
                        continue
                    Al = [B("Aa" + sfx, par), B("Ab" + sfx, par)]
                    Bl = [B("Ba" + sfx, par), B("Bb" + sfx, par)]
                    Tl = [B("Ta" + sfx, par), B("Tb" + sfx, par)]
                    D_(lambda e, A0=Al[0], E1=E1, be2=be2, KKm=KKm, cb=cb: e.scalar_tensor_tensor(out=A0[:], in0=E1[:], scalar=be2[:, cb:cb + 1], in1=KKm[:],
                                                                                               op0=ALU.mult, op1=ALU.mult), [E1, be2, KKm], [Al[0]])
                    QKd = B("QKd" + sfx, par)
                    G_(lambda e, QKd=QKd, QKTm=QKTm, E2=E2: e.tensor_tensor(out=QKd[:], in0=QKTm[:], in1=E2[:], op=ALU.mult), [QKTm, E2], [QKd])
                    pt = pM[cb]
                    P.tr(pt, pt[:, 0:128], Al[0], Al[0][:, :], cst, C(IDENT))
                    A_(lambda e, B0=Bl[0], pt=pt: e.activation(out=B0[:], in_=pt[:, 0:128], func=AF.Copy), [pt], [Bl[0]])
                    D_(lambda e, T0=Tl[0], B0=Bl[0]: e.tensor_tensor(out=T0[:], in0=C(IDENT), in1=B0[:], op=ALU.subtract), [cst, Bl[0]], [Tl[0]])
                    for l in range(DBG.get('nl', 5)):
                        Ac, Bc, An, Bn = Al[l % 2], Bl[l % 2], Al[(l + 1) % 2], Bl[(l + 1) % 2]
                        Tc, Tn = Tl[l % 2], Tl[(l + 1) % 2]
                        p1 = pM[cb]
                        P.mm(p1, p1[:, 0:128], Bc, Bc[:, :], Ac, Ac[:, :], True, True)
                        if l < 4:
                            P.mm(p1, p1[:, 128:256], Ac, Ac[:, :], Bc, Bc[:, :], True, True)
                        A_(lambda e, An=An, p1=p1: e.activation(out=An[:], in_=p1[:, 0:128], func=AF.Copy), [p1], [An])
                        if l < 4:
                            A_(lambda e, Bn=Bn, p1=p1: e.activation(out=Bn[:], in_=p1[:, 128:256], func=AF.Copy), [p1], [Bn])
                        P.mm(p1, p1[:, 256:384], An, An[:, :], Tc, Tc[:, :], True, True)
                        D_(lambda e, Tn=Tn, Tc=Tc, p1=p1: e.tensor_tensor(out=Tn[:], in0=p1[:, 256:384], in1=Tc[:], op=ALU.add), [Tc, p1], [Tn])
                    Tt = Tl[1]
                    if DBG.get('gcut', 9) < 6:
                        continue
                    RHv = B("RHv" + sfx, par); RHk = B("RHk" + sfx, par); kdec = B("kdec" + sfx, par)
                    D_(lambda e, RHv=RHv, Vh=V[cb], be2=be2, cb=cb: e.tensor_scalar(out=RHv[:], in0=Vh[:], scalar1=be2[:, cb:cb + 1], scalar2=None, op0=ALU.mult), [V[cb], be2], [RHv])
                    D_(lambda e, RHk=RHk, Kt=Kt, bk2=bk2, cb=cb: e.tensor_scalar(out=RHk[:], in0=Kt[:], scalar1=bk2[:, cb:cb + 1], scalar2=None, op0=ALU.mult), [Kt, bk2], [RHk])
                    D_(lambda e, kdec=kdec, Kt=Kt, kd2=kd2, cb=cb: e.tensor_scalar(out=kdec[:], in0=Kt[:], scalar1=kd2[:, cb:cb + 1], scalar2=None, op0=ALU.mult), [Kt, kd2], [kdec])
                    P.mm(pU, pU[:, 0:128], Tt, Tt[:, :], RHv, RHv[:, :], True, True)
                    P.mm(pU, pU[:, 128:256], RHk, RHk[:, :], Tt, Tt[:, :], True, True)
                    un = B("un" + sfx, par); wdT = B("wdT" + sfx, par)
                    A_(lambda e, un=un: e.activation(out=un[:], in_=pU[:, 0:128], func=AF.Copy), [pU], [un])
                    A_(lambda e, wdT=wdT: e.activation(out=wdT[:], in_=pU[:, 128:256], func=AF.Copy), [pU], [wdT])
                    if DBG.get('gcut', 9) < 7:
                        continue
                    vn = B("vn" + sfx, par); t1 = B("t1" + sfx, par)
                    Sb = S[(d, cb)]
                    for ch in ((0, 1) if d == 0 else (1, 0)):
                        r = slice(ch * 64, (ch + 1) * 64)
                        P.mm(pR, pR[r, 0:128], wdT, wdT[:, r], Sb, Sb[:, :], True, True)
                        D_(lambda e, vn=vn, un=un, r=r: e.scalar_tensor_tensor(out=vn[r, :], in0=pR[r, 0:128], scalar=-1.0, in1=un[r, :], op0=ALU.mult, op1=ALU.add), [un, pR], [vn])
                        P.mm(pR, pR[r, 128:256], qT, qT[:, r], Sb, Sb[:, :], True, True)
                        P.mm(pR, pR[r, 256:384], QKd, QKd[r, r], vn, vn[r, :], True, True)
                        A_(lambda e, t1=t1, r=r, qs2=qs2, cb=cb: e.activation(out=t1[r, :], in_=pR[r, 128:256], func=AF.Copy, scale=qs2[r, cb:cb + 1]), [pR, qs2], [t1])
                        D_(lambda e, o_t=o_t, t1=t1, r=r, cb=cb: e.tensor_tensor(out=o_t[r, cb * 128:(cb + 1) * 128], in0=pR[r, 256:384], in1=t1[r, :], op=ALU.add), [t1, pR], [o_t])
                        P.mm(pKVb, pKVb[:, kvo:kvo + 128], kdec, kdec[r, :], vn, vn[r, :], True, True)
                        col = cb * 2 + ch
                        D_(lambda e, Sb=Sb, eglb=eglb, col=col: e.tensor_scalar(out=Sb[:], in0=Sb[:], scalar1=eglb[:, col:col + 1], scalar2=None, op0=ALU.mult), [Sb, eglb], [Sb])
                        D_(lambda e, Sb=Sb: e.tensor_tensor(out=Sb[:], in0=pKVb[:, kvo:kvo + 128], in1=Sb[:], op=ALU.add), [Sb, pKVb], [Sb])
                if DBG.get('gcut', 9) < 8:
                    continue
                if dbg_d is not None and d == DBG.get("gdbg_d", 0):
                    for (bb, c0, wdt) in ((X[0], 0, 128), (X[1], 128, 128), (X[2], 256, 128), (X[3], 384, 128), (ab, 512, 8), (g2, 520, 2), (be2, 522, 2),
                                          (gcl, 524, 4), (eglb, 528, 4), (o_t, 532, 256), (bufs[("Tb0", par)], 788, 128), (bufs[("un0", par)], 916, 108)):
                        P.dma('sp', dbg_d, dbg_d[t0:t0 + 128, c0:c0 + wdt], bb, bb[:, 0:wdt], disjoint=True)
                if d == 0:
                    P.dma('sp', of_s, of_s[t0:t0 + 128, :], o_t, o_t[:], disjoint=True)
                else:
                    of_t = B("of_t", par, (128, 256))
                    P.dma('sp', of_t, of_t[:], of_s, of_s[t0:t0 + 128, :])
                    pz = pA[0]
                    for k in range(KC):
                        P.mm(pz, pz[:, 0:256], u, u[:, k * GW + t0 - glo:k * GW + t0 - glo + 128], wzb, wzb[:, k * 256:(k + 1) * 256], k == 0, k == KC - 1)
                    sgz = B("sgz", par, (128, 256)); zs = B("zs", par, (128, 256))
                    A_(lambda e, sgz=sgz, pz=pz: e.activation(out=sgz[:], in_=pz[:, 0:256], func=AF.Sigmoid), [pz], [sgz])
                    D_(lambda e, zs=zs, sgz=sgz, pz=pz: e.tensor_tensor(out=zs[:], in0=pz[:, 0:256], in1=sgz[:], op=ALU.mult), [pz, sgz], [zs])
                    D_(lambda e, o_t=o_t, of_t=of_t: e.tensor_tensor(out=o_t[:], in0=o_t[:], in1=of_t[:], op=ALU.add), [o_t, of_t], [o_t])
                    ss = B("ss", par, (128, 2)); junk = B("junk", par, (128, 128)); y = B("y", par, (128, 256))
                    for hl in range(2):
                        A_(lambda e, junk=junk, o_t=o_t, ss=ss, hl=hl: e.activation(out=junk[:], in_=o_t[:, hl * 128:(hl + 1) * 128], func=AF.Square, accum_out=ss[:, hl:hl + 1]),
                           [o_t], [junk, ss])
                    A_(lambda e, ss=ss: e.activation(out=ss[:], in_=ss[:], func=AF.Sqrt, bias=EPS, scale=1.0 / 128), [ss], [ss])
                    D_(lambda e, ss=ss: e.reciprocal(out=ss[:], in_=ss[:]), [ss], [ss])
                    for hl in range(2):
                        D_(lambda e, y=y, o_t=o_t, ss=ss, hl=hl: e.scalar_tensor_tensor(out=y[:, hl * 128:(hl + 1) * 128], in0=o_t[:, hl * 128:(hl + 1) * 128],
                                                                                      scalar=ss[:, hl:hl + 1], in1=nwb[:, :], op0=ALU.mult, op1=ALU.mult), [o_t, ss, nwb], [y])
                    D_(lambda e, y=y, zs=zs: e.tensor_tensor(out=y[:], in0=y[:], in1=zs[:], op=ALU.mult), [y, zs], [y])
                    yT = B("yT", par, (128, 256), BF16)
                    for hl in range(2):
                        pt = pA[1]
                        P.tr(pt, pt[:, 0:128], y, y[:, hl * 128:(hl + 1) * 128], cst, C(IDENT))
                        A_(lambda e, yT=yT, pt=pt, hl=hl: e.activation(out=yT[:, hl * 128:(hl + 1) * 128], in_=pt[:, 0:128], func=AF.Copy), [pt], [yT])
                        P.dma('sp', oT_d, oT_d[hl * 128:(hl + 1) * 128, t0:t0 + 128], yT, yT[:, hl * 128:(hl + 1) * 128], disjoint=True)
    cst_np, cind_np = gdn_consts()
    in_maps = []
    for c in range(NCORES):
        hv = [2 * c, 2 * c + 1]
        chunks = [w_in[:, c * 128:(c + 1) * 128], w_in[:, 1024 + c * 128:1024 + (c + 1) * 128],
                  w_in[:, 2048 + hv[0] * 128:2048 + (hv[0] + 1) * 128], w_in[:, 2048 + hv[1] * 128:2048 + (hv[1] + 1) * 128]]
        wnp = np.stack([x.reshape(KC, 128, 128) for x in chunks], 0).reshape(4 * KC, 128, 128)
        wz = w_in[:, 4096 + hv[0] * 128:4096 + (hv[1] + 1) * 128].reshape(KC, 128, 256)
        abcols = [6144 + kind * 32 + dd * 16 + h for kind in range(2) for dd in range(2) for h in hv]
        wab = w_in[:, abcols].reshape(KC, 128, 8)
        ch_idx = [np.arange(c * 128, (c + 1) * 128), 1024 + np.arange(c * 128, (c + 1) * 128),
                  2048 + np.arange(hv[0] * 128, (hv[0] + 1) * 128), 2048 + np.arange(hv[1] * 128, (hv[1] + 1) * 128)]
        cwn = np.concatenate([conv_w[:, idx].T for idx in ch_idx], 1)
        al = np.array([a_log[dd, h] for dd in range(2) for h in hv], np.float32)
        dtv = np.array([dt_bias[dd, h] for dd in range(2) for h in hv], np.float32)
        in_maps.append({"uT": np.ascontiguousarray(uT_all), "w": np.ascontiguousarray(wnp, dtype=np.float32),
                        "wz": np.ascontiguousarray(wz, dtype=np.float32), "wab": np.ascontiguousarray(wab, dtype=np.float32),
                        "cw": np.ascontiguousarray(cwn, dtype=np.float32), "cst": cst_np, "cind": cind_np,
                        "alog": _bc(al), "dtb": _bc(dtv), "nw": _bc(norm_w)})
    res = P.run(in_maps)
    if DBG.get("gdbg"):
        DBG["dbg_out"] = [res[c]["dbg"] for c in range(NCORES)]
    return np.concatenate([res[c]["oT"] for c in range(NCORES)], 0)


def _tok_index(c, LC):
    return np.concatenate([np.arange(CTX), CTX + c * LC + np.arange(LC)])


def _gather_T(per_core, LC, NT):
    cols = []
    for c in range(NCORES):
        a = np.asarray(per_core[c]).reshape(NT, 128, KC, 128).transpose(2, 1, 0, 3).reshape(D, NT * 128)
        cols.append(a[:, CTX:] if c > 0 else a)
    return np.ascontiguousarray(np.concatenate(cols, 1))


def kernel(x, c, ctx, c_ctx, ada_w, ada_b, ln_w, ln_b, gdn_w_in, gdn_conv_w, gdn_a_log, gdn_dt_bias,
           gdn_norm_w, gdn_w_out, diff_w_in, diff_lambda, diff_subln_w, diff_w_out, router_w, router_b,
           moe_w_gate_up, moe_b_gate_up, moe_w_down, moe_b_down):
    f32 = lambda a: np.asarray(a, dtype=np.float32)
    x, c, ctx, c_ctx = f32(x), f32(c), f32(ctx), f32(c_ctx)
    L = x.shape[1]
    LC = L // NCORES
    NT = (CTX + LC) // 128
    TT = CTX + L
    mods = launch_mods(c[0], c_ctx, f32(ada_w), f32(ada_b))

    def m(l, r, i):
        return mods[l, r, i * D:(i + 1) * D]
    h_cores = [np.concatenate([ctx[0], x[0, cc * LC:(cc + 1) * LC]], 0) for cc in range(NCORES)]
    partials = None
    prev = None
    for l in range(DEPTH):
        j = l // 2
        cur = dict(m0_lat=m(l, 0, 0), m1_lat=m(l, 0, 1), m0_ctx=m(l, 1, 0), m1_ctx=m(l, 1, 1))
        h_cores, uT = launch_tokA(h_cores, partials, prev, cur, NT)
        uT_all = _gather_T(uT, LC, NT)
        if l % 2 == 0:
            oT_all = launch_gdn(uT_all, f32(gdn_w_in[j]), f32(gdn_conv_w[j]), f32(gdn_a_log[j]), f32(gdn_dt_bias[j]), f32(gdn_norm_w[j]), TT)
            HK = 16
            wout = f32(gdn_w_out[j])
        else:
            lambda_init = 0.8 - 0.6 * math.exp(-0.3 * l)
            oT_all = launch_attn(uT_all, f32(diff_w_in[j]), f32(diff_lambda[j]), f32(diff_subln_w[j]), lambda_init, TT)
            HK = 8
            wout = f32(diff_w_out[j])
        oT_cores = []
        for cc in range(NCORES):
            a = oT_all[:, _tok_index(cc, LC)]
            oT_cores.append(np.ascontiguousarray(a.reshape(HK, 128, NT, 128).transpose(2, 1, 0, 3).reshape(NT, 128, HK * 128)))
        mod = dict(m2_lat=m(l, 0, 2), m2_ctx=m(l, 1, 2), lnw=f32(ln_w[l, 0]), lnb=f32(ln_b[l, 0]),
                   m3_lat=m(l, 0, 3), m4_lat=m(l, 0, 4), m3_ctx=m(l, 1, 3), m4_ctx=m(l, 1, 4))
        h_cores, vT, gates = launch_tokB(h_cores, oT_cores, wout, mod, f32(router_w[l]), f32(router_b[l]), NT, HK)
        vT_all = _gather_T(vT, LC, NT)
        gates_all = np.concatenate([gates[0]] + [gates[cc][CTX:] for cc in range(1, NCORES)], 0)
        parts = launch_moe(vT_all, gates_all, f32(moe_w_gate_up[l]), f32(moe_b_gate_up[l]), f32(moe_w_down[l]), f32(moe_b_down[l]), TT)
        partials = [np.stack([parts[r][_tok_index(cc, LC)] for r in range(NCORES)], 0) for cc in range(NCORES)]
        prev = dict(m5_lat=m(l, 0, 5), m5_ctx=m(l, 1, 5), lnw=f32(ln_w[l, 1]), lnb=f32(ln_b[l, 1]))
    h_cores, _ = launch_tokA(h_cores, partials, prev, None, NT)
    out = np.concatenate([h_cores[cc][CTX:] for cc in range(NCORES)], 0)[None]
    return np.ascontiguousarray(out, dtype=np.float32)
```
